# Optimizing a Trainium2 kernel written in Bass

```python
import jax, jax.numpy as jnp
from jax import lax
import numpy as np

D_MODEL = 1024
BATCH = 4
SEQ = 8192
DEPTH = 1

PLE_DIM = 256
MIX_WIDTH = D_MODEL
MLA_WIDTH = MIX_WIDTH // 2
CONV_WIDTH = MIX_WIDTH - MLA_WIDTH
N_HEADS = 8
V_HEAD_DIM = MLA_WIDTH // N_HEADS
QK_NOPE_DIM = 64
QK_ROPE_DIM = 32
Q_LORA = 3 * D_MODEL // 8
KV_LORA = D_MODEL // 4
CONV_K = 3
CONV_GROUPS = 8
N_GROUPS = 4
EXPERTS_PER_GROUP = 8
N_EXPERTS = N_GROUPS * EXPERTS_PER_GROUP
TOP_K = 2
EXPERT_FF = D_MODEL // 2
ROPE_BASE = 10000.0
EPS = 1e-6
Q_BLOCK = 128
MOE_BLOCK = 128
IN_SPLITS = (Q_LORA, KV_LORA, QK_ROPE_DIM, CONV_WIDTH, CONV_WIDTH, CONV_WIDTH)
IN_COLS = sum(IN_SPLITS)

kernel_name = "hybrid_mla_shortconv_hmoe_ple"


def rmsnorm(x, g):
    xf = x.astype(jnp.float32)
    y = xf * lax.rsqrt(jnp.mean(xf * xf, axis=-1, keepdims=True) + EPS)
    return (y * g.astype(jnp.float32)).astype(x.dtype)


def rope_tables(positions):
    inv_freq = ROPE_BASE ** (-jnp.arange(0, QK_ROPE_DIM, 2, dtype=jnp.float32) / QK_ROPE_DIM)
    ang = positions.astype(jnp.float32)[..., None] * inv_freq
    return jnp.cos(ang)[:, :, None, :], jnp.sin(ang)[:, :, None, :]


def apply_rope(x, cos, sin):
    half = QK_ROPE_DIM // 2
    x1, x2 = x[..., :half], x[..., half:]
    c, s = cos.astype(x.dtype), sin.astype(x.dtype)
    return jnp.concatenate([x1 * c - x2 * s, x2 * c + x1 * s], axis=-1)


def mla_mixer(c_q, c_kv, k_rope_raw, q_norm_g, w_uq, kv_norm_g, w_ukv, cos, sin):
    B, S, _ = c_q.shape
    q = jnp.einsum('bsr,rhd->bshd', rmsnorm(c_q, q_norm_g), w_uq)
    q_nope = q[..., :QK_NOPE_DIM]
    q_rope = apply_rope(q[..., QK_NOPE_DIM:], cos, sin)
    kv = jnp.einsum('bsr,rhd->bshd', rmsnorm(c_kv, kv_norm_g), w_ukv)
    k_nope, v = kv[..., :QK_NOPE_DIM], kv[..., QK_NOPE_DIM:]
    k_rope = apply_rope(k_rope_raw[:, :, None, :], cos, sin)[:, :, 0, :]
    scale = (QK_NOPE_DIM + QK_ROPE_DIM) ** -0.5
    outs = []
    for i in range(S // Q_BLOCK):
        q0, q1 = i * Q_BLOCK, (i + 1) * Q_BLOCK
        s = (jnp.einsum('bqhd,bkhd->bhqk', q_nope[:, q0:q1], k_nope[:, :q1])
             + jnp.einsum('bqhr,bkr->bhqk', q_rope[:, q0:q1], k_rope[:, :q1]))
        s = s.astype(jnp.float32) * scale
        mask = jnp.arange(q0, q1)[:, None] >= jnp.arange(q1)[None, :]
        s = jnp.where(mask, s, -jnp.inf)
        pr = jax.nn.softmax(s, axis=-1).astype(v.dtype)
        outs.append(jnp.einsum('bhqk,bkhd->bqhd', pr, v[:, :q1]))
    o = jnp.concatenate(outs, axis=1)
    return o.reshape(B, S, MLA_WIDTH)


def short_conv_mixer(b_gate, c_gate, u, conv_w):
    cu = c_gate * u
    y = lax.conv_general_dilated(cu, conv_w.astype(cu.dtype), window_strides=(1,),
                                 padding=((CONV_K - 1, 0),),
                                 dimension_numbers=('NWC', 'WIO', 'NWC'),
                                 feature_group_count=CONV_WIDTH)
    return b_gate * y


def hier_moe(xn, w_group_router, b_group_router, w_expert_router, b_expert_router,
             w_gate, w_up, w_down):
    B, S, D = xn.shape
    N = B * S
    xt = xn.reshape(N, D)
    xf = xt.astype(jnp.float32)
    g_prob = jax.nn.softmax(xf @ w_group_router.astype(jnp.float32)
                            + b_group_router.astype(jnp.float32), axis=-1)
    g_p, g_idx = lax.top_k(g_prob, 1)
    e_logits = (xf @ w_expert_router.astype(jnp.float32)
                + b_expert_router.astype(jnp.float32)).reshape(N, N_GROUPS, EXPERTS_PER_GROUP)
    e_logits = jnp.take_along_axis(e_logits, g_idx[:, :, None], axis=1)[:, 0]
    e_p, e_i = lax.top_k(jax.nn.softmax(e_logits, axis=-1), TOP_K)
    weights = g_p * (e_p / jnp.sum(e_p, axis=-1, keepdims=True))
    expert_ids = g_idx * EXPERTS_PER_GROUP + e_i

    A = N * TOP_K
    flat_e = expert_ids.reshape(A).astype(jnp.int32)
    flat_tok = jnp.repeat(jnp.arange(N, dtype=jnp.int32), TOP_K)
    flat_w = weights.reshape(A)
    order = jnp.argsort(flat_e, stable=True)
    se, stok, sw = flat_e[order], flat_tok[order], flat_w[order]
    counts = jnp.zeros((N_EXPERTS,), jnp.int32).at[flat_e].add(1)
    starts = jnp.cumsum(counts) - counts
    padded = (counts + MOE_BLOCK - 1) // MOE_BLOCK * MOE_BLOCK
    pad_ends = jnp.cumsum(padded)
    pad_starts = pad_ends - padded
    dest = pad_starts[se] + jnp.arange(A, dtype=jnp.int32) - starts[se]
    n_blocks = A // MOE_BLOCK + N_EXPERTS
    P = n_blocks * MOE_BLOCK
    row_tok = jnp.full((P,), N, jnp.int32).at[dest].set(stok)
    row_w = jnp.zeros((P,), jnp.float32).at[dest].set(sw)
    blk_e = jnp.minimum(jnp.searchsorted(pad_ends, jnp.arange(n_blocks, dtype=jnp.int32) * MOE_BLOCK,
                                         side='right'), N_EXPERTS - 1).astype(jnp.int32)
    x_pad = jnp.concatenate([xt, jnp.zeros((1, D), xt.dtype)], axis=0)
    xb = x_pad[row_tok].reshape(n_blocks, MOE_BLOCK, D)

    def expert_block(args):
        xblk, e = args
        hdn = jax.nn.silu(xblk @ w_gate[e]) * (xblk @ w_up[e])
        return hdn @ w_down[e]

    yb = lax.map(expert_block, (xb, blk_e)).reshape(P, D)
    y = jnp.zeros((N + 1, D), yb.dtype).at[row_tok].add(yb * row_w[:, None].astype(yb.dtype))
    return y[:N].reshape(B, S, D)


def setup_inputs(seed: int = 0) -> dict:
    key = jax.random.key(seed)
    ks = jax.random.split(key, 24)
    f32 = jnp.float32

    def nrm(k, shape, scale):
        return jax.random.normal(k, shape, f32) * scale

    def gain(k, shape):
        return 1.0 + 0.01 * jax.random.normal(k, shape, f32)

    x = jax.random.normal(ks[0], (BATCH, SEQ, D_MODEL), f32)
    p = jax.random.normal(ks[1], (DEPTH, BATCH, SEQ, PLE_DIM), f32)
    offs = jax.random.randint(ks[2], (BATCH, 1), 0, 1024, dtype=jnp.int32)
    positions = offs + jnp.arange(SEQ, dtype=jnp.int32)[None, :]
    return {
        "x": x,
        "p": p,
        "positions": positions,
        "attn_norm_g": gain(ks[3], (DEPTH, D_MODEL)),
        "w_in": nrm(ks[4], (DEPTH, D_MODEL, IN_COLS), D_MODEL ** -0.5),
        "q_norm_g": gain(ks[5], (DEPTH, Q_LORA)),
        "w_uq": nrm(ks[6], (DEPTH, Q_LORA, N_HEADS, QK_NOPE_DIM + QK_ROPE_DIM), Q_LORA ** -0.5),
        "kv_norm_g": gain(ks[7], (DEPTH, KV_LORA)),
        "w_ukv": nrm(ks[8], (DEPTH, KV_LORA, N_HEADS, QK_NOPE_DIM + V_HEAD_DIM), KV_LORA ** -0.5),
        "conv_w": nrm(ks[9], (DEPTH, CONV_K, 1, CONV_WIDTH), CONV_K ** -0.5),
        "w_out": nrm(ks[10], (DEPTH, MIX_WIDTH, D_MODEL), MIX_WIDTH ** -0.5),
        "moe_norm_g": gain(ks[11], (DEPTH, D_MODEL)),
        "w_group_router": nrm(ks[12], (DEPTH, D_MODEL, N_GROUPS), D_MODEL ** -0.5),
        "b_group_router": nrm(ks[13], (DEPTH, N_GROUPS), 0.01),
        "w_expert_router": nrm(ks[14], (DEPTH, D_MODEL, N_EXPERTS), D_MODEL ** -0.5),
        "b_expert_router": nrm(ks[15], (DEPTH, N_EXPERTS), 0.01),
        "w_gate": nrm(ks[16], (DEPTH, N_EXPERTS, D_MODEL, EXPERT_FF), D_MODEL ** -0.5),
        "w_up": nrm(ks[17], (DEPTH, N_EXPERTS, D_MODEL, EXPERT_FF), D_MODEL ** -0.5),
        "w_down": nrm(ks[18], (DEPTH, N_EXPERTS, EXPERT_FF, D_MODEL), EXPERT_FF ** -0.5),
        "ple_norm_g": gain(ks[19], (DEPTH, D_MODEL)),
        "w_ple_gate": nrm(ks[20], (DEPTH, D_MODEL, D_MODEL), D_MODEL ** -0.5),
        "b_ple_gate": nrm(ks[21], (DEPTH, D_MODEL), 0.01),
        "w_ple_proj": nrm(ks[22], (DEPTH, PLE_DIM, D_MODEL), PLE_DIM ** -0.5),
        "final_norm_g": gain(ks[23], (D_MODEL,)),
    }


def reference(x, p, positions, attn_norm_g, w_in, q_norm_g, w_uq, kv_norm_g, w_ukv, conv_w,
              w_out, moe_norm_g, w_group_router, b_group_router, w_expert_router,
              b_expert_router, w_gate, w_up, w_down, ple_norm_g, w_ple_gate, b_ple_gate,
              w_ple_proj, final_norm_g):
    cos, sin = rope_tables(positions)
    split_pts = [int(v) for v in np.cumsum(IN_SPLITS)[:-1]]
    h = x
    for i in range(DEPTH):
        xn = rmsnorm(h, attn_norm_g[i])
        z = xn @ w_in[i]
        c_q, c_kv, k_r, b_g, c_g, u = jnp.split(z, split_pts, axis=-1)
        o_mla = mla_mixer(c_q, c_kv, k_r, q_norm_g[i], w_uq[i], kv_norm_g[i], w_ukv[i], cos, sin)
        o_conv = short_conv_mixer(b_g, c_g, u, conv_w[i])
        h = h + jnp.concatenate([o_mla, o_conv], axis=-1) @ w_out[i]
        h = h + hier_moe(rmsnorm(h, moe_norm_g[i]), w_group_router[i], b_group_router[i],
                         w_expert_router[i], b_expert_router[i], w_gate[i], w_up[i], w_down[i])
        gate = jax.nn.sigmoid(rmsnorm(h, ple_norm_g[i]) @ w_ple_gate[i] + b_ple_gate[i])
        h = h + gate * (p[i] @ w_ple_proj[i])
    return rmsnorm(h, final_norm_g)
```

```python
import math
from contextlib import ExitStack
import numpy as np
import concourse.bass as bass
import concourse.mybir as mybir
from concourse.bass_utils import run_bass_kernel_spmd

F32 = mybir.dt.float32
BF16 = mybir.dt.bfloat16
I32 = mybir.dt.int32
AF = mybir.ActivationFunctionType
ALU = mybir.AluOpType
AX = mybir.AxisListType

D = 1024
S = 8192
NT = 4096
QL = 384
KVL = 256
NH = 8
CAP = 384
NE = 32
NSLOT = NE * CAP
EPS = 1e-6
TILES = ([0, 3, 4, 7, 8, 11, 12, 15], [1, 2, 5, 6, 9, 10, 13, 14])
SB_BASE = 16512
SB_END = 229376
PI = math.pi
C1 = 6.28125
C2 = 2.0 * math.pi - 6.28125
SCALE = 96.0 ** -0.5
BIG = 1.0e6


class Op:
    __slots__ = ("eng", "fn", "deps", "alldeps", "dma", "done", "needed", "idx", "amt", "cost", "lat", "bar")


class Prog:
    def __init__(self, nc):
        self.nc = nc
        self.ops = []
        self.lastw = {}
        self.readers = {}
        self.persist_top = SB_BASE
        self.top = SB_BASE
        self.n = 0
        self.last_eng = {}
        self.dma_since = []

    def sb(self, shape, dt, persist=False):
        sz = 1
        for s in shape[1:]:
            sz *= s
        sz *= mybir.dt.size(dt)
        sz = (sz + 63) // 64 * 64
        off = self.top
        self.top += sz
        if persist:
            assert self.persist_top == off, "persist alloc must precede phase allocs"
            self.persist_top = self.top
        assert self.top <= SB_END, ("SBUF overflow", self.top)
        self.n += 1
        return self.nc.alloc_sbuf_tensor_at("t%d" % self.n, list(shape), dt, offset=off)

    def phase_reset(self):
        self.barrier()
        self.top = self.persist_top

    def op(self, eng, fn, r=(), w=(), dma=False, cost=0.5, lat=0.0):
        o = Op()
        o.eng = eng
        o.fn = fn
        o.dma = dma
        o.done = None
        o.needed = dma
        o.idx = len(self.ops)
        o.amt = 16 if dma else 1
        o.cost = cost
        o.lat = lat
        o.bar = False
        deps = set()
        for x in r:
            lw = self.lastw.get(x)
            if lw is not None:
                deps.add((lw, 0))
        for x in w:
            lw = self.lastw.get(x)
            if lw is not None:
                deps.add((lw, 1))
            for rd in self.readers.get(x, ()):
                deps.add((rd, 1))
        need = set()
        alld = set()
        for A, kind in deps:
            if A is o:
                continue
            alld.add(A)
            if A.eng == eng and not A.dma and not dma:
                if eng == "pe":
                    continue
            need.add(A)
        o.deps = need
        o.alldeps = alld
        for x in w:
            self.lastw[x] = o
            self.readers[x] = []
        for x in r:
            if x not in w:
                self.readers.setdefault(x, []).append(o)
        self.ops.append(o)
        return o

    def barrier(self):
        o = Op()
        o.eng = "bar"
        o.fn = None
        o.dma = False
        o.done = None
        o.needed = False
        o.idx = len(self.ops)
        o.amt = 1
        o.cost = 0.0
        o.lat = 0.0
        o.bar = True
        o.deps = set()
        o.alldeps = set()
        self.ops.append(o)

    def schedule(self):
        import heapq
        ENG = ("pe", "act", "dve", "pool", "sp")
        segs = [[]]
        for o in self.ops:
            if o.bar:
                segs.append([])
            else:
                segs[-1].append(o)
        new_ops = []
        for si, seg in enumerate(segs):
            inseg = set(id(o) for o in seg)
            succ = {}
            indeg = {}
            for o in seg:
                d = [a for a in o.alldeps if id(a) in inseg]
                indeg[id(o)] = len(d)
                for a in d:
                    succ.setdefault(id(a), []).append(o)
            bl = {}
            for o in reversed(seg):
                m_ = 0.0
                for c in succ.get(id(o), ()):
                    v_ = bl[id(c)] + 0.3
                    if v_ > m_:
                        m_ = v_
                bl[id(o)] = m_ + o.cost + o.lat
            finish = {}
            efree = {e: 0.0 for e in ENG}
            pend = {e: [] for e in ENG}
            avail = {e: [] for e in ENG}

            def rtime(o):
                t = 0.0
                for a in o.alldeps:
                    f = finish.get(id(a))
                    if f is None:
                        continue
                    f = f + (0.1 if (a.eng == o.eng and not a.dma) else 0.3)
                    if f > t:
                        t = f
                return t

            for o in seg:
                if indeg[id(o)] == 0:
                    heapq.heappush(pend[o.eng], (0.0, -bl[id(o)], o.idx, o))
            order = []
            nleft = len(seg)
            while nleft:
                best = None
                for e in ENG:
                    while pend[e] and pend[e][0][0] <= efree[e]:
                        rt, pr, ix, o = heapq.heappop(pend[e])
                        heapq.heappush(avail[e], (pr, ix, o))
                    if avail[e]:
                        cand = (efree[e], avail[e][0][0], e, 0)
                    elif pend[e]:
                        cand = (pend[e][0][0], pend[e][0][1], e, 1)
                    else:
                        continue
                    if best is None or cand < best:
                        best = cand
                st, ix, e, which = best
                if which == 0:
                    pr, ix, o = heapq.heappop(avail[e])
                else:
                    rt, pr, ix, o = heapq.heappop(pend[e])
                efree[e] = st + (o.cost if not o.dma else (0.2 if e == "sp" else 1.2))
                finish[id(o)] = st + o.cost + o.lat
                order.append(o)
                nleft -= 1
                for c in succ.get(id(o), ()):
                    indeg[id(c)] -= 1
                    if indeg[id(c)] == 0:
                        heapq.heappush(pend[c.eng], (rtime(c), -bl[id(c)], c.idx, c))
            new_ops.extend(order)
            if si < len(segs) - 1 or True:
                lasts = {}
                dmas = []
                for o in order:
                    if o.dma:
                        dmas.append(o)
                    else:
                        lasts[o.eng] = o
                for e in ENG:
                    b = Op()
                    b.eng = e
                    b.fn = None
                    b.dma = False
                    b.done = None
                    b.needed = False
                    b.idx = -1
                    b.amt = 1
                    b.cost = 0.0
                    b.lat = 0.0
                    b.bar = False
                    b.deps = set(dmas) | set(v for k, v in lasts.items() if k != e)
                    b.alldeps = set()
                    for a in b.deps:
                        a.needed = True
                    new_ops.append(b)
        for o in new_ops:
            for a in o.deps:
                a.needed = True
        self.ops = new_ops

    def emit(self, stack):
        nc = self.nc
        EPOCH = 3000
        NDS = 12
        comp = {}
        sems = {}

        def getsem(key):
            if key not in sems:
                sems[key] = stack.enter_context(nc.semaphore("s_%s_%s" % key))
            return sems[key]

        self.schedule()
        cnt = {}
        dcnt = {}
        dval = {}
        dlast = {}
        for o in self.ops:
            if o.dma:
                k = dcnt.get(o.eng, 0)
                dcnt[o.eng] = k + 1
                key = ("d" + o.eng, k % NDS)
                prev = dlast.get(key)
                if prev is not None:
                    o.deps.add(prev)
                dlast[key] = o
                v = dval.get(key, 0) + 16
                dval[key] = v
                o.done = (getsem(key), v)
            elif o.needed:
                c = cnt.get(o.eng, 0)
                cnt[o.eng] = c + 1
                key = (o.eng, c // EPOCH)
                o.done = (getsem(key), c % EPOCH + 1)
        per = {e: [] for e in ("pe", "act", "dve", "pool", "sp")}
        for o in self.ops:
            per[o.eng].append(o)
        ops = self.ops
        engs = {"pe": "tensor", "act": "scalar", "dve": "vector", "pool": "gpsimd", "sp": "sync"}

        def replay(name):
            def run(e):
                waited = {}
                if name == "pool":
                    reg = e.alloc_register("bchk")
                    e.reg_mov(reg, NSLOT - 1)
                    self.breg = reg
                for o in per[name]:
                    ws = {}
                    for a in o.deps:
                        sem, val = a.done
                        k = id(sem)
                        if k not in ws or ws[k][1] < val:
                            ws[k] = (sem, val)
                    for k, (sem, val) in ws.items():
                        if waited.get(k, 0) >= val:
                            continue
                        waited[k] = val
                        e.wait_ge(sem, val)
                    if o.fn is not None:
                        ins = o.fn(e)
                        if o.done is not None:
                            ins.then_inc(o.done[0], o.amt)
            return run

        with nc.Block() as block:
            for name, attr in engs.items():
                getattr(block, attr)(replay(name))


def build_program():
    nc = bass.Bass("TRN2", target_bir_lowering=False)
    P = Prog(nc)
    stack = ExitStack()

    def din(name, shape, dt=F32):
        return nc.dram_tensor(name, list(shape), dt, kind="ExternalInput").ap()

    def dscr(name, shape, dt):
        return nc.dram_tensor(name, list(shape), dt, kind="Internal").ap()

    x_all = din("x_all", [S, D])
    x_own = din("x_own", [NT, D])
    x_halo = din("x_halo", [16, D])
    p_own = din("p_own", [NT, 256])
    pos_all = din("pos_all", [32, S], I32)
    pos_own = din("pos_own", [32, NT], I32)
    kthr_d = din("kthr", [128, 64])
    iota_d = din("iota", [128, 512])
    ident_d = din("ident", [128, 128])
    utri_d = din("utri", [128, 128])
    rconst_d = din("rconst", [32, 4])
    esel_d = din("esel", [65, 64])
    capb_d = din("capb", [128, 64])
    g_attn_d = din("g_attn", [128, D])
    g_moe_d = din("g_moe", [128, D])
    g_ple_d = din("g_ple", [128, D])
    g_fin_d = din("g_fin", [128, D])
    b_ple_d = din("b_ple", [128, D])
    b_rt_d = din("b_rt", [128, 36])
    gq_d = din("gq_t", [128, 3])
    gkv_d = din("gkv_t", [128, 2])
    cw_d = din("cw_t", [128, 12])
    w_in_d = din("w_in", [D, 2208])
    w_krp_d = din("w_krp", [D, 32])
    w_uqn_d = din("w_uqn", [QL, NH * 64])
    w_uqr_d = din("w_uqr", [QL, NH * 32])
    w_uqp_d = din("w_uqp", [QL, NH * 32])
    w_ukn_d = din("w_ukn", [KVL, NH * 64])
    w_uv_d = din("w_uv", [KVL, NH * 64])
    w_out_d = din("w_out", [D, D])
    w_rt_d = din("w_rt", [D, 36])
    w_gate_d = din("w_gate", [NE, D, 512])
    w_up_d = din("w_up", [NE, D, 512])
    w_down_d = din("w_down", [NE, 512, D])
    w_pg_d = din("w_pg", [D, D])
    w_pp_d = din("w_pp", [256, D])
    out_d = nc.dram_tensor("out", [NT, D], F32, kind="ExternalOutput").ap()

    ocv_s = dscr("ocv_s", [4, 128, NT], BF16)
    om_s = dscr("om_s", [NH, 64, NT], BF16)
    h1_s = dscr("h1_s", [NT, D], F32)
    xe_s = dscr("xe_s", [NSLOT, D], BF16)
    ye_s = dscr("ye_s", [NSLOT, D], F32)

    PS = [stack.enter_context(nc.psum_tensor("ps%d" % i, [128, 512], F32)) for i in range(8)]

    def psb(i):
        return PS[i][:, :].bitcast(BF16)

    def fsz(ap):
        n = 1
        for d_ in ap.shape[1:]:
            n *= int(d_)
        return n

    def dma(out, in_, r, w, eng="sp"):
        nbytes = fsz(out) * int(out.shape[0]) * mybir.dt.size(out.dtype)
        lat = 2.0 + nbytes / 150e3
        if eng == "sp":
            P.op("sp", lambda e: e.dma_start(out=out, in_=in_), r, w, dma=True, cost=0.2, lat=lat)
        else:
            P.op("pool", lambda e: e.dma_start(out=out, in_=in_), r, w, dma=True, cost=1.2, lat=lat + 1.0)

    def mm(out, lhsT, rhs, st, sp_, r, w):
        c = max(fsz(rhs), 64) / 1900.0 + 0.03
        if rhs.dtype == F32:
            c *= 4
        P.op("pe", lambda e: e.matmul(out, lhsT, rhs, start=st, stop=sp_), r, w, cost=c)

    def tr(out, in_, ident, r, w):
        c = 0.3 if in_.dtype == F32 else 0.1
        P.op("pe", lambda e: e.transpose(out, in_, ident), r, w, cost=c)

    def act(out, in_, func, r, w, bias=0.0, scale=1.0, accum=None):
        c = 0.25 + fsz(in_) / 1300.0
        if accum is None:
            P.op("act", lambda e: e.activation(out, in_, func, bias=bias, scale=scale), r, w, cost=c)
        else:
            P.op("act", lambda e: e.activation(out, in_, func, bias=bias, scale=scale, accum_out=accum), r, w, cost=c + 0.15)

    def vcost(eng, n):
        if eng == "pool":
            return 1.0 + n / 250.0
        return 0.12 + n / 900.0

    def ts(eng, out, in0, s1, s2, op0, op1, r, w):
        c = vcost(eng, fsz(in0))
        if op1 is None:
            P.op(eng, lambda e: e.tensor_scalar(out, in0, s1, None, op0), r, w, cost=c)
        else:
            P.op(eng, lambda e: e.tensor_scalar(out, in0, s1, s2, op0, op1), r, w, cost=c)

    def tt(eng, out, in0, in1, op, r, w):
        P.op(eng, lambda e: e.tensor_tensor(out, in0, in1, op), r, w, cost=vcost(eng, fsz(in0)))

    def stt(eng, out, in0, sc, in1, op0, op1, r, w):
        P.op(eng, lambda e: e.scalar_tensor_tensor(out, in0, sc, in1, op0, op1), r, w, cost=vcost(eng, fsz(in0)))

    def cp(eng, out, in_, r, w):
        if eng == "act":
            P.op("act", lambda e: e.copy(out, in_), r, w, cost=0.25 + fsz(in_) / 1300.0)
        else:
            P.op(eng, lambda e: e.tensor_copy(out, in_), r, w, cost=vcost(eng, fsz(in_)))

    def recip(out, in_, r, w):
        P.op("dve", lambda e: e.reciprocal(out, in_), r, w, cost=vcost("dve", fsz(in_)))

    def memset(eng, ap, val, w):
        P.op(eng, lambda e: e.memset(ap, val), (), w, cost=vcost(eng, fsz(ap)))

    def redmax(out, in_, r, w):
        P.op("dve", lambda e: e.reduce_max(out, in_, AX.X), r, w, cost=vcost("dve", fsz(in_)))

    def redsum(out, in_, r, w):
        P.op("dve", lambda e: e.reduce_sum(out, in_, AX.X), r, w, cost=vcost("dve", fsz(in_)))

    def drive(*pairs):
        live = [[g, n] for g, n in pairs]
        while live:
            for it in list(live):
                g, n = it
                for _ in range(n):
                    try:
                        next(g)
                    except StopIteration:
                        live.remove(it)
                        break

    def exhaust(g):
        for _ in g:
            pass

    ident_f = P.sb([128, 128], F32, True)
    ident_b = P.sb([128, 128], BF16, True)
    utri_b = P.sb([128, 128], BF16, True)
    ones_b = P.sb([128, 128], BF16, True)
    rconst = P.sb([32, 4], F32, True)
    stats = P.sb([128, 16], F32, True)
    tmpc = P.sb([128, 128], F32, True)
    epsc = P.sb([128, 1], F32, True)

    dma(ident_f[:, :], ident_d, (), ["ident_f"])
    cp("dve", ident_b[:, :], ident_f[:, :], ["ident_f"], ["ident_b"])
    dma(tmpc[:, :], utri_d, (), ["tmpc"])
    cp("dve", utri_b[:, :], tmpc[:, :], ["tmpc"], ["utri_b"])
    memset("pool", ones_b[:, :], 1.0, ["ones_b"])
    memset("pool", epsc[:, :], EPS, ["epsc"])
    dma(rconst[:, :], rconst_d, (), ["rconst"])

    def wchunks(dram2d, c0, c1):
        return dram2d[:, c0:c1].rearrange("(k p) c -> p k c", p=128)

    def norm_block(tag, xb, nrows, gbc, xn, dt_out_is_bf=True):
        ss = stats[0:nrows, 0:1]
        sd = stats[0:nrows, 1:2]
        rs = stats[0:nrows, 2:3]
        act(junk[0:nrows, :], xb, AF.Square, [tag + "xb"], ["junk", "st_ss"], accum=ss)
        act(sd, ss, AF.Ln, ["st_ss", "epsc"], ["st_sd"], bias=epsc[0:nrows, 0:1], scale=1.0 / D)
        act(rs, sd, AF.Exp, ["st_sd"], ["st_rs"], scale=-0.5)
        stt("dve", xn, xb, rs, gbc[0:nrows, :], ALU.mult, ALU.mult, [tag + "xb", "st_rs", "gbc"], [tag + "xn"])

    def transposes_bf(tag, xn, nrows, psi, dst, nk=8):
        pv = psb(psi)
        for k in range(nk):
            tr(pv[:, k * nrows:(k + 1) * nrows], xn[:, k * 128:(k + 1) * 128], ident_b[0:nrows, 0:nrows],
               [tag + "xn", "ident_b"], ["ps%d" % psi])
        return pv[:, 0:nk * nrows].rearrange("p (k t) -> p k t", k=nk)

    def rope_tables(tag, pos_i, n, cos_o, sin_o, tmp):
        pf, ang, a2, kq, kf, r1, wv = tmp
        cp("dve", pf[:, 0:n], pos_i, [tag + "pos"], [tag + "pf"])
        ts("dve", ang[:, 0:n], pf[:, 0:n], rconst[:, 0:1], None, ALU.mult, None, [tag + "pf", "rconst"], [tag + "ang"])
        ts("dve", kq[:, 0:n], ang[:, 0:n], 1.0 / (2 * PI), None, ALU.mult, None, [tag + "ang"], [tag + "kq"])
        cp("dve", kf[:, 0:n], kq[:, 0:n], [tag + "kq"], [tag + "kf"])
        stt("dve", r1[:, 0:n], kf[:, 0:n], -C1, ang[:, 0:n], ALU.mult, ALU.add, [tag + "kf", tag + "ang"], [tag + "r1"])
        stt("dve", r1[:, 0:n], kf[:, 0:n], -C2, r1[:, 0:n], ALU.mult, ALU.add, [tag + "kf", tag + "r1"], [tag + "r1"])
        ts("dve", wv[:, 0:n], r1[:, 0:n], PI, -2 * PI, ALU.is_gt, ALU.mult, [tag + "r1"], [tag + "wv"])
        tt("dve", r1[:, 0:n], r1[:, 0:n], wv[:, 0:n], ALU.add, [tag + "r1", tag + "wv"], [tag + "r1"])
        ts("dve", r1[:, 0:n], r1[:, 0:n], PI, -PI, ALU.min, ALU.max, [tag + "r1"], [tag + "r1"])
        act(sin_o, r1[:, 0:n], AF.Sin, [tag + "r1", "rconst"], [tag + "sin"], scale=rconst[:, 1:2])
        stt("dve", a2[:, 0:n], r1[:, 0:n], -1.0, r1[:, 0:n], ALU.mult, ALU.max, [tag + "r1"], [tag + "a2"])
        act(cos_o, a2[:, 0:n], AF.Sin, [tag + "a2", "rconst"], [tag + "cos"], bias=rconst[:, 2:3], scale=-1.0)

    const_top = P.persist_top
    CKV = P.sb([128, 2, S], BF16, True)
    CQ = P.sb([128, 3, NT], BF16, True)
    KT = [P.sb([96, S], BF16, True) for _ in range(2)]
    COSQ = P.sb([32, NT], F32, True)
    SINQ = P.sb([32, NT], F32, True)
    persist_mark = P.persist_top

    junk = P.sb([128, D], BF16)
    gbc = P.sb([128, D], F32)
    XB = [P.sb([128, D], F32) for _ in range(2)]
    XN = [P.sb([128, D], BF16) for _ in range(2)]
    XT = [P.sb([128, 8, 512], BF16) for _ in range(2)]
    WCV = P.sb([128, 8, 1536], BF16)
    cw = P.sb([128, 12], F32)
    XTh = P.sb([128, 8, 16], BF16)
    CUH = P.sb([128, 4, 16], F32)
    usb = P.sb([128, 512], F32)
    CU = [P.sb([128, 514], F32) for _ in range(2)]
    acc = P.sb([128, 512], F32)
    OCb = [P.sb([128, 512], BF16) for _ in range(2)]

    dma(gbc[:, :], g_attn_d, (), ["gbc"])
    dma(cw[:, :], cw_d, (), ["cw"])
    dma(WCV[:, :, :], wchunks(w_in_d, 672, 2208), (), ["WCV"], eng="pool")
    dma(XB[0][0:16, :], x_halo, (), ["X0xb"])
    norm_block("X0", XB[0][0:16, :], 16, gbc, XN[0][0:16, :])
    pv = transposes_bf("X0", XN[0][0:16, :], 16, 0, None)
    cp("dve", XTh[:, :, :], pv, ["ps0"], ["XTh"])
    for cc in range(4):
        for k in range(8):
            mm(PS[1][:, cc * 16:(cc + 1) * 16], WCV[:, k, 512 + cc * 128:512 + (cc + 1) * 128], XTh[:, k, :],
               k == 0, k == 7, ["WCV", "XTh"], ["ps1"])
        for k in range(8):
            mm(PS[2][:, cc * 16:(cc + 1) * 16], WCV[:, k, 1024 + cc * 128:1024 + (cc + 1) * 128], XTh[:, k, :],
               k == 0, k == 7, ["WCV", "XTh"], ["ps2"])
    cp("act", usb[:, 0:64], PS[2][:, 0:64], ["ps2"], ["usb"])
    tt("dve", CUH[:, :, :].rearrange("p a b -> p (a b)"), PS[1][:, 0:64], usb[:, 0:64], ALU.mult, ["ps1", "usb"], ["CUH"])

    def own_tile_norm(j, gname_unused, xt, tagp, src):
        for bi in range(4):
            blk = j * 4 + bi
            s = blk % 2
            tg = "X%d" % s
            dma(XB[s][:, :], src[blk * 128:(blk + 1) * 128, :], (), [tg + "xb"])
            norm_block(tg, XB[s][:, :], 128, gbc, XN[s][:, :])
            pv_ = transposes_bf(tg, XN[s][:, :], 128, s, None)
            cp("act" if bi % 2 else "dve", xt[:, :, bi * 128:(bi + 1) * 128], pv_, ["ps%d" % s], ["XT%d" % (j % 2)])
            yield

    def bc_tail(j):
        xt = XT[j % 2]
        xtn = "XT%d" % (j % 2)
        for cc in range(4):
            pb0 = 2 + 3 * (cc % 2)
            for gi, base in enumerate((0, 512, 1024)):
                for k in range(8):
                    mm(PS[pb0 + gi][:, :], WCV[:, k, base + cc * 128:base + (cc + 1) * 128], xt[:, k, :],
                       k == 0, k == 7, ["WCV", xtn], ["ps%d" % (pb0 + gi)])
            cu = CU[cc % 2]
            cun = "CU%d" % (cc % 2)
            us_ = usb2[cc % 2]
            usn = "usb%d" % (cc % 2)
            ac_ = acc2[cc % 2]
            acn = "acc%d" % (cc % 2)
            cp("act", us_[:, :], PS[pb0 + 2][:, :], ["ps%d" % (pb0 + 2)], [usn])
            tt("dve", cu[:, 2:514], PS[pb0 + 1][:, :], us_[:, :], ALU.mult, ["ps%d" % (pb0 + 1), usn], [cun])
            cp("dve", cu[:, 0:2], CUH[:, cc, 2 * j:2 * j + 2], ["CUH"], [cun + "h"])
            ts("dve", ac_[:, :], cu[:, 2:514], cw[:, cc * 3 + 2:cc * 3 + 3], None, ALU.mult, None, [cun, "cw"], [acn])
            stt("dve", ac_[:, :], cu[:, 1:513], cw[:, cc * 3 + 1:cc * 3 + 2], ac_[:, :], ALU.mult, ALU.add,
                [cun, cun + "h", "cw", acn], [acn])
            stt("dve", ac_[:, :], cu[:, 0:512], cw[:, cc * 3:cc * 3 + 1], ac_[:, :], ALU.mult, ALU.add,
                [cun, cun + "h", "cw", acn], [acn])
            ob = OCb[cc % 2]
            tt("dve", ob[:, :], ac_[:, :], PS[pb0][:, :], ALU.mult, [acn, "ps%d" % pb0], ["OCb%d" % (cc % 2)])
            dma(ocv_s[cc, :, j * 512:(j + 1) * 512], ob[:, :], ["OCb%d" % (cc % 2)], ["ocv_s"], eng="pool")
            yield

    usb2 = [usb, P.sb([128, 512], F32)]
    acc2 = [acc, P.sb([128, 512], F32)]
    exhaust(own_tile_norm(0, None, XT[0], "B", x_own))
    for j in range(8):
        gens = []
        if j + 1 < 8:
            gens.append((own_tile_norm(j + 1, None, XT[(j + 1) % 2], "B", x_own), 1))
        gens.append((bc_tail(j), 1))
        drive(*gens)

    P.phase_reset()
    junk = P.sb([128, D], BF16)
    gbc = P.sb([128, D], F32)
    XB = [P.sb([128, D], F32) for _ in range(2)]
    XN = [P.sb([128, D], BF16) for _ in range(2)]
    XT = [P.sb([128, 8, 512], BF16) for _ in range(2)]
    WKV = P.sb([128, 8, 256], BF16)
    WKR = P.sb([128, 8, 32], BF16)
    WKP = P.sb([128, 8, 32], BF16)
    WQI = P.sb([128, 8, QL], BF16)
    gkv = P.sb([128, 2], F32)
    gq = P.sb([128, 3], F32)
    SQ = P.sb([128, 3, 512], BF16)
    STD = P.sb([128, 512], F32)
    RS = P.sb([128, 512], F32)
    posi = P.sb([32, 512], I32)
    rtmp = [P.sb([32, 512], F32) for _ in range(3)] + [P.sb([32, 512], I32)] + [P.sb([32, 512], F32) for _ in range(3)]
    cosk = P.sb([32, 512], F32)
    sink = P.sb([32, 512], F32)
    t1 = P.sb([32, 512], F32)
    t2 = P.sb([32, 512], F32)

    dma(gbc[:, :], g_attn_d, (), ["gbc"])
    dma(gkv[:, :], gkv_d, (), ["gkv"])
    dma(gq[:, :], gq_d, (), ["gq0"])
    ts("dve", gq[:, :], gq[:, :], SCALE, None, ALU.mult, None, ["gq0"], ["gq"])
    dma(WKV[:, :, :], wchunks(w_in_d, 384, 640), (), ["WKV"], eng="pool")
    dma(WKR[:, :, :], wchunks(w_in_d, 640, 672), (), ["WKR"], eng="pool")
    dma(WKP[:, :, :], wchunks(w_krp_d, 0, 32), (), ["WKP"], eng="pool")
    dma(WQI[:, :, :], wchunks(w_in_d, 0, 384), (), ["WQI"], eng="pool")

    def a_tail(t):
        xt = XT[t % 2]
        xtn = "XT%d" % (t % 2)
        for c in range(2):
            for k in range(8):
                mm(PS[2 + c][:, :], WKV[:, k, c * 128:(c + 1) * 128], xt[:, k, :], k == 0, k == 7, ["WKV", xtn], ["ps%d" % (2 + c)])
        for k in range(8):
            mm(PS[4][0:32, :], WKR[:, k, :], xt[:, k, :], k == 0, k == 7, ["WKR", xtn], ["ps4"])
        for k in range(8):
            mm(PS[5][0:32, :], WKP[:, k, :], xt[:, k, :], k == 0, k == 7, ["WKP", xtn], ["ps5"])
        for c in range(2):
            act(SQ[:, c, :], PS[2 + c][:, :], AF.Square, ["ps%d" % (2 + c)], ["SQ%d" % c])
        for c in range(2):
            mm(PS[6][:, :], ones_b[:, :], SQ[:, c, :], c == 0, c == 1, ["ones_b", "SQ%d" % c], ["ps6"])
        act(STD[:, :], PS[6][:, :], AF.Ln, ["ps6", "epsc"], ["STD"], bias=epsc[:, 0:1], scale=1.0 / KVL)
        yield
        act(RS[:, :], STD[:, :], AF.Exp, ["STD"], ["RS"], scale=-0.5)
        yield
        for c in range(2):
            stt("dve", CKV[:, c, t * 512:(t + 1) * 512], PS[2 + c][:, :], gkv[:, c:c + 1], RS[:, :], ALU.mult, ALU.mult,
                ["ps%d" % (2 + c), "gkv", "RS"], ["CKV"])
        dma(posi[:, :], pos_all[:, t * 512:(t + 1) * 512], (), ["Kpos"])
        yield
        rope_tables("K", posi[:, :], 512, cosk[:, :], sink[:, :], rtmp)
        yield
        tt("dve", t1[:, :], PS[4][0:32, :], cosk[:, :], ALU.mult, ["ps4", "Kcos"], ["t1"])
        yield
        tt("dve", t2[:, :], PS[5][0:32, :], sink[:, :], ALU.mult, ["ps5", "Ksin"], ["t2"])
        yield
        tt("pool", KT[0][0:32, t * 512:(t + 1) * 512], t1[:, :], t2[:, :], ALU.add, ["t1", "t2"], ["KT0r"])
        yield
        tt("pool", KT[1][0:32, t * 512:(t + 1) * 512], t1[:, :], t2[:, :], ALU.add, ["t1", "t2"], ["KT1r"])
        yield

    def q_tail(j):
        xt = XT[j % 2]
        xtn = "XT%d" % (j % 2)
        for c in range(3):
            for k in range(8):
                mm(PS[2 + c][:, :], WQI[:, k, c * 128:(c + 1) * 128], xt[:, k, :], k == 0, k == 7, ["WQI", xtn], ["ps%d" % (2 + c)])
        for c in range(3):
            act(SQ[:, c, :], PS[2 + c][:, :], AF.Square, ["ps%d" % (2 + c)], ["SQ%d" % c])
        for c in range(3):
            mm(PS[6][:, :], ones_b[:, :], SQ[:, c, :], c == 0, c == 2, ["ones_b", "SQ%d" % c], ["ps6"])
        act(STD[:, :], PS[6][:, :], AF.Ln, ["ps6", "epsc"], ["STD"], bias=epsc[:, 0:1], scale=1.0 / QL)
        yield
        act(RS[:, :], STD[:, :], AF.Exp, ["STD"], ["RS"], scale=-0.5)
        yield
        for c in range(3):
            stt("dve", CQ[:, c, j * 512:(j + 1) * 512], PS[2 + c][:, :], gq[:, c:c + 1], RS[:, :], ALU.mult, ALU.mult,
                ["ps%d" % (2 + c), "gq", "RS"], ["CQ"])
        dma(posi[:, :], pos_own[:, j * 512:(j + 1) * 512], (), ["Kpos"])
        yield
        rope_tables("K", posi[:, :], 512, COSQ[:, j * 512:(j + 1) * 512], SINQ[:, j * 512:(j + 1) * 512], rtmp)
        yield

    fronts = [(t, x_all) for t in range(16)] + [(j, x_own) for j in range(8)]
    exhaust(own_tile_norm(fronts[0][0], None, XT[0], "A", fronts[0][1]))
    for i_ in range(24):
        gens = []
        if i_ + 1 < 24:
            gens.append((own_tile_norm(fronts[i_ + 1][0], None, XT[(i_ + 1) % 2], "A", fronts[i_ + 1][1]), 1))
        if i_ < 16:
            gens.append((a_tail(i_), 3))
        else:
            gens.append((q_tail(i_ - 16), 3))
        drive(*gens)

    P.phase_reset()
    Vb = [P.sb([128, 64, 65], BF16) for _ in range(2)]
    QT = [P.sb([96, NT], BF16) for _ in range(2)]
    WQn = P.sb([128, 3, NH, 96], BF16)
    WQr = P.sb([128, 3, NH, 32], BF16)
    WQp = P.sb([128, 3, NH, 32], BF16)
    WKn = P.sb([128, 2, NH, 96], BF16)
    WV = P.sb([128, 2, NH, 64], BF16)
    PT = [P.sb([128, 512], BF16) for _ in range(3)]
    Osb = [P.sb([65, 512], F32) for _ in range(2)]
    rden = P.sb([64, 512], F32)
    OMb = [P.sb([64, 512], BF16) for _ in range(2)]
    qt1 = P.sb([32, 512], F32)
    qt2 = P.sb([32, 512], F32)
    kthr = P.sb([128, 64], F32)
    iota = P.sb([128, 512], F32)
    esel = P.sb([65, 64], F32)

    dma(kthr[:, :], kthr_d, (), ["kthr"])
    dma(iota[:, :], iota_d, (), ["iota"])
    dma(esel[:, :], esel_d, (), ["esel"])
    memset("pool", WQn[:, :, :, :], 0.0, ["WQn"])
    memset("pool", WKn[:, :, :, :], 0.0, ["WKn"])
    for b_ in range(2):
        memset("pool", Vb[b_][:, :, 64:65], 1.0, ["Vb%done" % b_])
    def dma_w4(dst, src2d, nch, lo, hi, name):
        for c_ in range(nch):
            dma(dst[:, c_, :, lo:hi], src2d[c_ * 128:(c_ + 1) * 128, :].rearrange("p (h d) -> p h d", h=NH), (), [name], eng="pool")

    dma_w4(WQn, w_uqn_d, 3, 32, 96, "WQn")
    dma_w4(WQr, w_uqr_d, 3, 0, 32, "WQr")
    dma_w4(WQp, w_uqp_d, 3, 0, 32, "WQp")
    dma_w4(WKn, w_ukn_d, 2, 32, 96, "WKn")
    dma_w4(WV, w_uv_d, 2, 0, 64, "WV")

    def gen_units(h):
        hb = h % 2
        kb_, vb_, qb_ = KT[hb], Vb[hb], QT[hb]
        kn, vn, qn = "KT%dn" % hb, "Vb%d" % hb, "QT%d" % hb
        units = []

        def ku(t):
            for c in range(2):
                mm(PS[5][0:96, :], WKn[:, c, h, :], CKV[:, c, t * 512:(t + 1) * 512], c == 0, c == 1, ["WKn", "CKV"], ["ps5"])
            cp("dve", kb_[32:64, t * 512:(t + 1) * 512], PS[5][32:64, :], ["ps5"], [kn + "a"])
            cp("dve", kb_[64:96, t * 512:(t + 1) * 512], PS[5][64:96, :], ["ps5"], [kn + "b"])

        def vu(g8):
            for i8 in range(8):
                blk = g8 * 8 + i8
                for c in range(2):
                    mm(PS[6][:, i8 * 64:(i8 + 1) * 64], CKV[:, c, blk * 128:(blk + 1) * 128], WV[:, c, h, :], c == 0, c == 1,
                       ["CKV", "WV"], ["ps6"])
            cp("dve", vb_[:, g8 * 8:(g8 + 1) * 8, 0:64], PS[6][:, :].rearrange("p (a b) -> p a b", a=8), ["ps6"], [vn])

        def qu(j):
            sl = slice(j * 512, (j + 1) * 512)
            for c in range(3):
                mm(PS[5][0:96, :], WQn[:, c, h, :], CQ[:, c, sl], c == 0, c == 2, ["WQn", "CQ"], ["ps5"])
            for c in range(3):
                mm(PS[6][0:32, :], WQr[:, c, h, :], CQ[:, c, sl], c == 0, c == 2, ["WQr", "CQ"], ["ps6"])
            for c in range(3):
                mm(PS[7][0:32, :], WQp[:, c, h, :], CQ[:, c, sl], c == 0, c == 2, ["WQp", "CQ"], ["ps7"])
            cp("dve", qb_[32:64, sl], PS[5][32:64, :], ["ps5"], [qn + "a"])
            cp("dve", qb_[64:96, sl], PS[5][64:96, :], ["ps5"], [qn + "b"])
            tt("dve", qt1[:, :], PS[6][0:32, :], COSQ[:, sl], ALU.mult, ["ps6", "Kcos"], ["qt1"])
            tt("dve", qt2[:, :], PS[7][0:32, :], SINQ[:, sl], ALU.mult, ["ps7", "Ksin"], ["qt2"])
            tt("pool", qb_[0:32, sl], qt1[:, :], qt2[:, :], ALU.add, ["qt1", "qt2"], [qn + "r"])

        for t in range(16):
            units.append(lambda t=t: ku(t))
        for g8 in range(8):
            units.append(lambda g8=g8: vu(g8))
        for j in range(8):
            units.append(lambda j=j: qu(j))
        return units

    for u in gen_units(0):
        u()
    cnt_sl = 0
    LA = 2
    for h in range(NH):
        hb = h % 2
        kb_, vb_, qb_ = KT[hb], Vb[hb], QT[hb]
        kn, vn, qn = "KT%dn" % hb, "Vb%d" % hb, "QT%d" % hb
        items = [(j, kb) for j in range(8) for kb in range(8 * (j + 1))]
        n_it = len(items)
        nxt = gen_units(h + 1) if h + 1 < NH else []
        deferred = {}
        slot_ob = {}
        for i in range(n_it + LA + 4):
            if i < n_it:
                j, kb = items[i]
                sl = slice(j * 512, (j + 1) * 512)
                sb_ = i % 3
                mm(PS[sb_][:, :], kb_[0:96, kb * 128:(kb + 1) * 128], qb_[0:96, sl], True, True,
                   [kn + "a", kn + "b", "KT%dr" % hb, qn + "a", qn + "b", qn + "r"], ["ps%d" % sb_])
                pt = PT[sb_]
                ptn = "PT%d" % sb_
                act(pt[:, :], PS[sb_][:, :], AF.Exp, ["ps%d" % sb_], [ptn])
                if kb >= 8 * j:
                    col = j * 8 + (kb - 8 * j)
                    stt("dve", pt[:, :], iota[:, :], kthr[:, col:col + 1], pt[:, :], ALU.is_ge, ALU.mult,
                        ["iota", "kthr", ptn], [ptn])
            if LA <= i < n_it + LA:
                j, kb = items[i - LA]
                sl = slice(j * 512, (j + 1) * 512)
                sb_ = (i - LA) % 3
                pt = PT[sb_]
                ptn = "PT%d" % sb_
                if kb == 0:
                    slot_ob[j] = cnt_sl
                    cnt_sl += 1
                cs_ = slot_ob[j]
                ob = 3 + (cs_ % 2)
                obn = "ps%d" % ob
                nkb = 8 * (j + 1)
                mm(PS[ob][0:65, :], vb_[:, kb, :], pt[:, :], kb == 0, kb == nkb - 1, [vn, vn + "one", ptn], [obn])
                if kb == nkb - 1:
                    os_ = Osb[cs_ % 2]
                    osn = "Osb%d" % (cs_ % 2)
                    cp("dve", os_[:, :], PS[ob][0:65, :], [obn], [osn])

                    def fin(os_=os_, osn=osn, cs_=cs_, sl=sl, h=h):
                        mm(PS[7][0:64, :], esel[:, :], os_[:, :], True, True, ["esel", osn], ["ps7"])
                        recip(rden[:, :], PS[7][0:64, :], ["ps7"], ["rden"])
                        omb = OMb[cs_ % 2]
                        tt("pool", omb[:, :], os_[0:64, :], rden[:, :], ALU.mult, [osn, "rden"], ["OMb%d" % (cs_ % 2)])
                        dma(om_s[h, :, sl], omb[:, :], ["OMb%d" % (cs_ % 2)], ["om_s"], eng="pool")
                    deferred.setdefault(i + 4, []).append(fin)
            for fn_ in deferred.pop(i, []):
                fn_()
            if nxt and i >= 8 and i % 8 == 0:
                nxt.pop(0)()
        for k_ in sorted(deferred):
            for fn_ in deferred[k_]:
                fn_()
        while nxt:
            nxt.pop(0)()

    P.barrier()
    P.persist_top = const_top
    P.top = const_top
    SL = P.sb([128, 32, 2], I32, True)
    WTS = P.sb([128, 32, 2], F32, True)
    CB = P.sb([128, 32], F32, True)
    LIM = P.sb([128, 32], F32, True)
    persist2 = P.persist_top

    junk = P.sb([128, D], BF16)
    gbc = P.sb([128, D], F32)
    XB = [P.sb([128, D], F32) for _ in range(2)]
    WOm = P.sb([128, 4, D], BF16)
    WOc = P.sb([128, 4, D], BF16)
    WR = P.sb([128, 8, 36], F32)
    brt = P.sb([128, 36], F32)
    OMs = [P.sb([128, 4, 512], BF16) for _ in range(2)]
    OCs = [P.sb([128, 4, 512], BF16) for _ in range(2)]
    H1b = [P.sb([128, D], F32) for _ in range(2)]
    XNFb = [P.sb([128, D], F32) for _ in range(2)]
    XN2 = [P.sb([128, D], BF16) for _ in range(4)]
    XNFTb = [P.sb([128, 8, 128], F32) for _ in range(2)]
    Rb = [P.sb([128, 256], F32) for _ in range(2)]
    A32b = [P.sb([128, 32], BF16) for _ in range(2)]
    sli = [P.sb([128, 2], I32) for _ in range(4)]

    dma(gbc[:, :], g_moe_d, (), ["gbc"])
    dma(brt[:, :], b_rt_d, (), ["brt"])
    dma(WR[:, :, :], wchunks(w_rt_d, 0, 36), (), ["WR"])
    dma(WOm[:, :, :], w_out_d[0:512, :].rearrange("(h p) c -> p h c", p=128), (), ["WOm"], eng="pool")
    dma(WOc[:, :, :], w_out_d[512:1024, :].rearrange("(k p) c -> p k c", p=128), (), ["WOc"], eng="pool")
    dma(CB[:, :], capb_d[:, 0:32], (), ["CB"])
    dma(LIM[:, :], capb_d[:, 32:64], (), ["LIM"])


    Lb = [P.sb([128, 36], F32) for _ in range(2)]
    junkD = [junk, P.sb([128, D], BF16)]

    def d_front(blk):
        j = blk // 4
        bi = blk % 4
        oms = OMs[j % 2]
        ocs = OCs[j % 2]
        if bi == 0:
            dma(oms[:, :, :], om_s.rearrange("(c two) p t -> (two p) c t", two=2)[:, :, j * 512:(j + 1) * 512], ["om_s"], ["OMs%d" % (j % 2)])
            dma(ocs[:, :, :], ocv_s[:, :, j * 512:(j + 1) * 512].rearrange("c p t -> p c t"), ["ocv_s"], ["OCs%d" % (j % 2)])
        s = blk % 2
        XNF = XNFb[s]
        XNFT = XNFTb[s]
        junk = junkD[s]
        tsl = slice(bi * 128, (bi + 1) * 128)
        dma(XB[s][:, :], x_own[blk * 128:(blk + 1) * 128, :], (), ["Dxb%d" % s])
        for half in range(2):
            pbi = half if blk % 2 == 0 else 6 + half
            pb_ = PS[pbi]
            cs = slice(half * 512, (half + 1) * 512)
            for hh in range(4):
                mm(pb_[:, :], oms[:, hh, tsl], WOm[:, hh, cs], hh == 0, False, ["OMs%d" % (j % 2), "WOm"], ["ps%d" % pbi])
            for cc in range(4):
                mm(pb_[:, :], ocs[:, cc, tsl], WOc[:, cc, cs], False, cc == 3, ["OCs%d" % (j % 2), "WOc"], ["ps%d" % pbi])
            tt("dve", H1b[s][:, cs], XB[s][:, cs], pb_[:, :], ALU.add, ["Dxb%d" % s, "ps%d" % pbi], ["H1b%d" % s])
        dma(h1_s[blk * 128:(blk + 1) * 128, :], H1b[s][:, :], ["H1b%d" % s], ["h1_s"], eng="pool")
        yield
        ss = stats[:, 8 * s + 0:8 * s + 1]
        sd = stats[:, 8 * s + 1:8 * s + 2]
        rs_ = stats[:, 8 * s + 2:8 * s + 3]
        act(junk[:, :], H1b[s][:, :], AF.Square, ["H1b%d" % s], ["junk" + str(s), "st_ss" + str(s)], accum=ss)
        act(sd, ss, AF.Ln, ["st_ss" + str(s), "epsc"], ["st_sd" + str(s)], bias=epsc[:, 0:1], scale=1.0 / D)
        act(rs_, sd, AF.Exp, ["st_sd" + str(s)], ["st_rs" + str(s)], scale=-0.5)
        stt("dve", XNF[:, :], H1b[s][:, :], rs_, gbc[:, :], ALU.mult, ALU.mult, ["H1b%d" % s, "st_rs" + str(s), "gbc"], ["XNF" + str(s)])
        cp("act", XN2[blk % 4][:, :], XNF[:, :], ["XNF" + str(s)], ["XN2%d" % (blk % 4)])
        yield
        for k in range(8):
            pst = PS[2 + k // 4]
            tr(pst[:, (k % 4) * 128:(k % 4 + 1) * 128], XNF[:, k * 128:(k + 1) * 128], ident_f[:, :],
               ["XNF" + str(s), "ident_f"], ["ps%d" % (2 + k // 4)])
        cp("act", XNFT[:, 0:4, :], PS[2][:, :].rearrange("p (k t) -> p k t", k=4), ["ps2"], ["XNFTa" + str(s)])
        cp("dve", XNFT[:, 4:8, :], PS[3][:, :].rearrange("p (k t) -> p k t", k=4), ["ps3"], ["XNFTb" + str(s)])
        yield
        for k in range(8):
            mm(PS[4][:, 0:36], XNFT[:, k, :], WR[:, k, :], k == 0, k == 7, ["XNFTa" + str(s), "XNFTb" + str(s), "WR"], ["ps4"])
        tt("dve", Lb[s][:, :], PS[4][:, 0:36], brt[:, :], ALU.add, ["ps4", "brt"], ["rL%d" % s])
        yield

    def d_tail(blk):
        s = blk % 2
        R = Rb[s]
        A32 = A32b[s]

        def rr(a, b):
            return R[:, a:b]
        pc = 64 * s
        L = Lb[s][:, :]
        gmax = rr(36, 37)
        redmax(gmax, L[:, 0:4], ["rL%d" % s], ["rgmax" + str(s)])
        yield
        G = rr(40, 44)
        ts("dve", G, L[:, 0:4], gmax, None, ALU.is_equal, None, ["rL%d" % s, "rgmax" + str(s)], ["rG" + str(s)])
        yield
        ngmax = rr(37, 38)
        ts("dve", ngmax, gmax, -1.0, None, ALU.mult, None, ["rgmax" + str(s)], ["rngmax" + str(s)])
        yield
        gsum = rr(38, 39)
        act(rr(44, 48), L[:, 0:4], AF.Exp, ["rL%d" % s, "rngmax" + str(s)], ["rgexp" + str(s), "rgsum" + str(s)], bias=ngmax, accum=gsum)
        yield
        gp = rr(39, 40)
        recip(gp, gsum, ["rgsum" + str(s)], ["rgp" + str(s)])
        yield
        el = rr(48, 56)
        ts("dve", el, L[:, 4:12], G[:, 0:1], None, ALU.mult, None, ["rL%d" % s, "rG" + str(s)], ["rel" + str(s)])
        yield
        for g_ in range(1, 4):
            stt("dve", el, L[:, 4 + 8 * g_:12 + 8 * g_], G[:, g_:g_ + 1], el, ALU.mult, ALU.add, ["rL%d" % s, "rG" + str(s), "rel" + str(s)], ["rel" + str(s)])
        m1 = rr(56, 57)
        redmax(m1, el, ["rel" + str(s)], ["rm1" + str(s)])
        yield
        E1 = rr(64, 72)
        ts("dve", E1, el, m1, None, ALU.is_equal, None, ["rel" + str(s), "rm1" + str(s)], ["rE1" + str(s)])
        yield
        el2 = rr(72, 80)
        stt("dve", el2, E1, -1.0e30, el, ALU.mult, ALU.add, ["rE1" + str(s), "rel" + str(s)], ["rel2" + str(s)])
        yield
        m2 = rr(57, 58)
        redmax(m2, el2, ["rel2" + str(s)], ["rm2" + str(s)])
        yield
        E2 = rr(80, 88)
        ts("dve", E2, el2, m2, None, ALU.is_equal, None, ["rel2" + str(s), "rm2" + str(s)], ["rE2" + str(s)])
        yield
        dd = rr(58, 59)
        tt("dve", dd, m2, m1, ALU.subtract, ["rm2" + str(s), "rm1" + str(s)], ["rdd" + str(s)])
        yield
        ed = rr(59, 60)
        act(ed, dd, AF.Exp, ["rdd" + str(s)], ["red" + str(s)])
        yield
        den = rr(60, 61)
        ts("dve", den, ed, 1.0, None, ALU.add, None, ["red" + str(s)], ["rden_" + str(s)])
        yield
        rdn = rr(61, 62)
        recip(rdn, den, ["rden_" + str(s)], ["rrdn" + str(s)])
        yield
        w1 = rr(62, 63)
        tt("dve", w1, gp, rdn, ALU.mult, ["rgp" + str(s), "rrdn" + str(s)], ["rw1" + str(s)])
        yield
        w2 = rr(63, 64)
        tt("dve", w2, w1, ed, ALU.mult, ["rw1" + str(s), "red" + str(s)], ["rw2" + str(s)])
        yield
        A8 = rr(88, 96)
        tt("dve", A8, E1, E2, ALU.add, ["rE1" + str(s), "rE2" + str(s)], ["rA8" + str(s)])
        yield
        for g_ in range(4):
            ts("dve", A32[:, g_ * 8:(g_ + 1) * 8], A8, G[:, g_:g_ + 1], None, ALU.mult, None, ["rA8" + str(s), "rG" + str(s)], ["A32" + str(s)])
        mm(PS[5][:, pc:pc + 32], utri_b[:, :], A32[:, :], True, True, ["utri_b", "A32" + str(s)], ["ps5" + str(s)])
        yield
        mm(PS[5][:, pc + 32:pc + 64], ones_b[:, :], A32[:, :], True, True, ["ones_b", "A32" + str(s)], ["ps5" + str(s)])
        yield
        POSB = rr(96, 128)
        tt("dve", POSB, PS[5][:, pc:pc + 32], CB[:, :], ALU.add, ["ps5" + str(s), "CB"], ["rPOSB" + str(s)])
        yield
        tt("dve", CB[:, :], CB[:, :], PS[5][:, pc + 32:pc + 64], ALU.add, ["ps5" + str(s), "CB", "rPOSB" + str(s)], ["CB"])
        yield
        ovf = rr(128, 160)
        tt("dve", ovf, POSB, LIM[:, :], ALU.is_ge, ["rPOSB" + str(s), "LIM"], ["rovf" + str(s)])
        yield
        stt("dve", POSB, ovf, BIG, POSB, ALU.mult, ALU.add, ["rovf" + str(s), "rPOSB" + str(s)], ["rPOSB" + str(s)])
        yield
        PG = rr(160, 168)
        ts("dve", PG, POSB[:, 0:8], G[:, 0:1], None, ALU.mult, None, ["rPOSB" + str(s), "rG" + str(s)], ["rPG" + str(s)])
        yield
        for g_ in range(1, 4):
            stt("dve", PG, POSB[:, 8 * g_:8 * g_ + 8], G[:, g_:g_ + 1], PG, ALU.mult, ALU.add, ["rPOSB" + str(s), "rG" + str(s), "rPG" + str(s)], ["rPG" + str(s)])
        slf = rr(176, 178)
        tmp8 = rr(168, 176)
        tt("dve", tmp8, E1, PG, ALU.mult, ["rE1" + str(s), "rPG" + str(s)], ["rtmp8" + str(s)])
        yield
        redsum(slf[:, 0:1], tmp8, ["rtmp8" + str(s)], ["rslf0" + str(s)])
        yield
        tt("dve", tmp8, E2, PG, ALU.mult, ["rE2" + str(s), "rPG" + str(s), "rslf0" + str(s)], ["rtmp8" + str(s)])
        yield
        redsum(slf[:, 1:2], tmp8, ["rtmp8" + str(s)], ["rslf1" + str(s)])
        yield
        s4 = blk % 4
        cp("dve", sli[s4][:, :], slf, ["rslf0" + str(s), "rslf1" + str(s)], ["sli%d" % s4])
        yield
        cp("dve", SL[:, blk, :], sli[s4][:, :], ["sli%d" % s4], ["SL"])
        yield
        okm = rr(178, 180)
        ts("dve", okm, slf, float(NSLOT) - 0.5, None, ALU.is_lt, None, ["rslf0" + str(s), "rslf1" + str(s)], ["rokm" + str(s)])
        yield
        tt("dve", WTS[:, blk, 0:1], w1, okm[:, 0:1], ALU.mult, ["rw1" + str(s), "rokm" + str(s)], ["WTS"])
        yield
        tt("dve", WTS[:, blk, 1:2], w2, okm[:, 1:2], ALU.mult, ["rw2" + str(s), "rokm" + str(s)], ["WTS"])
        yield
        for k_ in range(2):
            P.op("pool", (lambda e, s4=s4, k_=k_: e.indirect_dma_start(
                out=xe_s, out_offset=bass.IndirectOffsetOnAxis(ap=sli[s4][:, k_:k_ + 1], axis=0),
                in_=XN2[s4][:, :], in_offset=None, bounds_check=P.breg, oob_is_err=False)),
                ["sli%d" % s4, "XN2%d" % s4], ["xe_s"], dma=True, cost=1.2, lat=6.0)


    exhaust(d_front(0))
    for blk in range(32):
        gens = []
        if blk + 1 < 32:
            gens.append((d_front(blk + 1), 1))
        gens.append((d_tail(blk), 14))
        drive(*gens)

    P.persist_top = persist2
    P.phase_reset()
    WG = [P.sb([128, 8, 512], BF16) for _ in range(2)]
    WU = [P.sb([128, 8, 512], BF16) for _ in range(2)]
    WD = [P.sb([128, 4, D], BF16) for _ in range(2)]
    SG = [P.sb([128, 8, 512], F32) for _ in range(2)]
    SU = [P.sb([128, 8, 512], F32) for _ in range(2)]
    SD = [P.sb([128, 4, D], F32) for _ in range(2)]
    XEb = [P.sb([128, D], BF16) for _ in range(3)]
    XTe = [P.sb([128, 8, CAP], BF16) for _ in range(2)]
    hT = [P.sb([128, 4, CAP], BF16) for _ in range(2)]
    sg = [P.sb([128, CAP], F32) for _ in range(2)]
    Yb = [P.sb([128, D], F32) for _ in range(2)]
    ycnt = 0
    def wload(e_):
        eb = e_ % 2
        dma(SG[eb][:, :, :], w_gate_d[e_].rearrange("(k p) f -> p k f", p=128), (), ["SG%d" % eb])
        dma(SU[eb][:, :, :], w_up_d[e_].rearrange("(k p) f -> p k f", p=128), (), ["SU%d" % eb])
        dma(SD[eb][:, :, :], w_down_d[e_].rearrange("(k p) f -> p k f", p=128), (), ["SD%d" % eb])

    wload(0)
    for e_ in range(NE):
        eb = e_ % 2
        if e_ + 1 < NE:
            wload(e_ + 1)
        for q4 in range(4):
            cp("dve", WG[eb][:, 2 * q4:2 * q4 + 2, :], SG[eb][:, 2 * q4:2 * q4 + 2, :], ["SG%d" % eb], ["WG%d" % eb])
            cp("act", WU[eb][:, 2 * q4:2 * q4 + 2, :], SU[eb][:, 2 * q4:2 * q4 + 2, :], ["SU%d" % eb], ["WU%d" % eb])
            cp("act" if q4 % 2 else "dve", WD[eb][:, q4, :], SD[eb][:, q4, :], ["SD%d" % eb], ["WD%d" % eb])
        xte = XTe[eb]
        for b3 in range(3):
            xb_ = XEb[b3]
            r0 = e_ * CAP + b3 * 128
            dma(xb_[:, :], xe_s[r0:r0 + 128, :], ["xe_s"], ["XEb%d" % b3])
            pv = psb(b3 % 2)
            for k in range(8):
                tr(pv[:, k * 128:(k + 1) * 128], xb_[:, k * 128:(k + 1) * 128], ident_b[:, :], ["XEb%d" % b3, "ident_b"],
                   ["ps%d" % (b3 % 2)])
            cp("act" if b3 % 2 else "dve", xte[:, :, b3 * 128:(b3 + 1) * 128],
               pv[:, 0:1024].rearrange("p (k t) -> p k t", k=8), ["ps%d" % (b3 % 2)], ["XTe%d" % eb])
        for fc in range(4):
            pg = 2 + (fc % 2) * 2
            for k in range(8):
                mm(PS[pg][:, 0:CAP], WG[eb][:, k, fc * 128:(fc + 1) * 128], xte[:, k, :], k == 0, k == 7,
                   ["WG%d" % eb, "XTe%d" % eb], ["ps%d" % pg])
            for k in range(8):
                mm(PS[pg + 1][:, 0:CAP], WU[eb][:, k, fc * 128:(fc + 1) * 128], xte[:, k, :], k == 0, k == 7,
                   ["WU%d" % eb, "XTe%d" % eb], ["ps%d" % (pg + 1)])
            act(sg[fc % 2][:, :], PS[pg][:, 0:CAP], AF.Silu, ["ps%d" % pg], ["sg%d" % (fc % 2)])
            tt("dve", hT[eb][:, fc, :], sg[fc % 2][:, :], PS[pg + 1][:, 0:CAP], ALU.mult, ["sg%d" % (fc % 2), "ps%d" % (pg + 1)],
               ["hT%d" % eb])
        for b3 in range(3):
            yb = Yb[ycnt % 2]
            ybn = "Yb%d" % (ycnt % 2)
            for half in range(2):
                pb_ = 6 + half
                for fc in range(4):
                    mm(PS[pb_][:, :], hT[eb][:, fc, b3 * 128:(b3 + 1) * 128], WD[eb][:, fc, half * 512:(half + 1) * 512],
                       fc == 0, fc == 3, ["hT%d" % eb, "WD%d" % eb], ["ps%d" % pb_])
                cp("act" if half else "dve", yb[:, half * 512:(half + 1) * 512], PS[pb_][:, :], ["ps%d" % pb_], [ybn])
            r0 = e_ * CAP + b3 * 128
            dma(ye_s[r0:r0 + 128, :], yb[:, :], [ybn], ["ye_s"], eng="pool")
            ycnt += 1

    P.phase_reset()
    junk = P.sb([128, D], BF16)
    gple = P.sb([128, D], F32)
    gfin = P.sb([128, D], F32)
    bple = P.sb([128, D], F32)
    WPG = P.sb([128, 8, D], BF16)
    WPP = P.sb([128, 2, D], BF16)
    H1c = [P.sb([128, D], F32) for _ in range(4)]
    Y1 = [P.sb([128, D], F32) for _ in range(4)]
    Y2 = [P.sb([128, D], F32) for _ in range(4)]
    Pb = [P.sb([128, 256], F32) for _ in range(4)]
    Pbb = P.sb([128, 256], BF16)
    XN3 = P.sb([128, D], BF16)
    XT3 = P.sb([128, 8, 128], BF16)
    PT3 = P.sb([128, 2, 128], BF16)
    tg_ = P.sb([128, D], F32)
    OUTb = [P.sb([128, D], F32) for _ in range(2)]
    negh = P.sb([128, 1], F32)
    memset("pool", negh[:, :], -0.5, ["negh"])
    dma(gple[:, :], g_ple_d, (), ["gple"])
    dma(gfin[:, :], g_fin_d, (), ["gfin"])
    bplb = P.sb([1, D], BF16)
    dma(bplb[:, :], b_ple_d[0:1, :], (), ["bplb"], eng="pool")
    dma(WPG[:, :, :], w_pg_d.rearrange("(k p) c -> p k c", p=128), (), ["WPG"], eng="pool")
    dma(WPP[:, :, :], w_pp_d.rearrange("(k p) c -> p k c", p=128), (), ["WPP"], eng="pool")
    for s in range(4):
        memset("pool", Y1[s][:, :], 0.0, ["Y1%d" % s])
        memset("pool", Y2[s][:, :], 0.0, ["Y2%d" % s])
    XT3b = [XT3] + [P.sb([128, 8, 128], BF16) for _ in range(3)]
    PT3b = [PT3] + [P.sb([128, 2, 128], BF16) for _ in range(3)]
    XN3b = [XN3, P.sb([128, D], BF16)]
    Pbb2 = [Pbb, P.sb([128, 256], BF16)]
    tg2 = [tg_, P.sb([128, D], F32)]
    junk2 = [junk, P.sb([128, D], BF16)]

    def f_a(blk):
        s = blk % 4
        s2 = blk % 4
        pz = blk % 2
        XN3 = XN3b[pz]
        Pbb = Pbb2[pz]
        junk = junk2[pz]
        h = H1c[s]
        hn = "H1c%d" % s
        dma(h[:, :], h1_s[blk * 128:(blk + 1) * 128, :], ["h1_s"], [hn])
        dma(Pb[s][:, :], p_own[blk * 128:(blk + 1) * 128, :], (), ["Pb%d" % s])
        for k_, Yk in enumerate((Y1, Y2)):
            yn = "Y%d%d" % (k_ + 1, s)
            P.op("pool", (lambda e, s=s, k_=k_, Yk=Yk, blk=blk: e.indirect_dma_start(
                out=Yk[s][:, :], out_offset=None, in_=ye_s,
                in_offset=bass.IndirectOffsetOnAxis(ap=SL[:, blk, k_:k_ + 1], axis=0),
                bounds_check=P.breg, oob_is_err=False)), ["SL", "ye_s"], [yn], dma=True, cost=1.2, lat=8.0)
        yield
        stt("dve", h[:, :], Y1[s][:, :], WTS[:, blk, 0:1], h[:, :], ALU.mult, ALU.add, ["Y1%d" % s, "WTS", hn], [hn])
        stt("dve", h[:, :], Y2[s][:, :], WTS[:, blk, 1:2], h[:, :], ALU.mult, ALU.add, ["Y2%d" % s, "WTS", hn], [hn])
        yield
        ss = stats[:, 8 * pz + 0:8 * pz + 1]
        sd = stats[:, 8 * pz + 1:8 * pz + 2]
        rs_ = stats[:, 8 * pz + 2:8 * pz + 3]
        act(junk[:, :], h[:, :], AF.Square, [hn], ["junk" + str(pz), "st_ss" + str(pz)], accum=ss)
        ts("pool", sd, ss, 1.0 / D, EPS, ALU.mult, ALU.add, ["st_ss" + str(pz)], ["st_sd" + str(pz)])
        tt("pool", rs_, sd, negh[:, 0:1], ALU.pow, ["st_sd" + str(pz), "negh"], ["st_rs" + str(pz)])
        stt("dve", XN3[:, :], h[:, :], rs_, gple[:, :], ALU.mult, ALU.mult, [hn, "st_rs" + str(pz), "gple"], ["XN3" + str(pz)])
        yield
        pv = psb(0)
        for k in range(8):
            tr(pv[:, k * 128:(k + 1) * 128], XN3[:, k * 128:(k + 1) * 128], ident_b[:, :], ["XN3" + str(pz), "ident_b"], ["ps0"])
        cp("act", XT3b[s2][:, :, :], pv[:, 0:1024].rearrange("p (k t) -> p k t", k=8), ["ps0"], ["XT3%d" % s2])
        yield
        cp("act", Pbb[:, :], Pb[s][:, :], ["Pb%d" % s], ["Pbb" + str(pz)])
        pv1 = psb(1)
        for k in range(2):
            tr(pv1[:, k * 128:(k + 1) * 128], Pbb[:, k * 128:(k + 1) * 128], ident_b[:, :], ["Pbb" + str(pz), "ident_b"], ["ps1"])
        cp("dve", PT3b[s2][:, :, :], pv1[:, 0:256].rearrange("p (k t) -> p k t", k=2), ["ps1"], ["PT3%d" % s2])
        yield

    def f_b(blk):
        s = blk % 2
        s3 = blk % 4
        pz = blk % 2
        tg_ = tg2[pz]
        junk = junk2[pz]
        h = H1c[s3]
        hn = "H1c%d" % s3
        for half in range(2):
            cs = slice(half * 512, (half + 1) * 512)
            pg_ = (2 + half) if blk % 2 == 0 else (6 + half)
            pp_ = 4 + half
            for k in range(8):
                mm(PS[pg_][:, :], XT3b[s3][:, k, :], WPG[:, k, cs], k == 0, False, ["XT3%d" % s3, "WPG"], ["ps%d" % pg_])
            mm(PS[pg_][:, :], ones_b[0:1, :], bplb[0:1, cs], False, True, ["ones_b", "bplb"], ["ps%d" % pg_])
            for k in range(2):
                mm(PS[pp_][:, :], PT3b[s3][:, k, :], WPP[:, k, cs], k == 0, k == 1, ["PT3%d" % s3, "WPP"], ["ps%d" % pp_])
            yield
            act(tg_[:, cs], PS[pg_][:, :], AF.Tanh, ["ps%d" % pg_], ["tg%d_%d" % (half, pz)], scale=0.5)
            stt("dve", tg_[:, cs], tg_[:, cs], 1.0, PS[pp_][:, :], ALU.add, ALU.mult, ["tg%d_%d" % (half, pz), "ps%d" % pp_], ["tg%d_%d" % (half, pz)])
            stt("dve", h[:, cs], tg_[:, cs], 0.5, h[:, cs], ALU.mult, ALU.add, [hn, "tg%d_%d" % (half, pz)], [hn])
            yield
        ss2 = stats[:, 8 * pz + 4:8 * pz + 5]
        sd2 = stats[:, 8 * pz + 5:8 * pz + 6]
        rs2 = stats[:, 8 * pz + 6:8 * pz + 7]
        act(junk[:, :], h[:, :], AF.Square, [hn], ["junk" + str(pz), "st_ss2" + str(pz)], accum=ss2)
        ts("pool", sd2, ss2, 1.0 / D, EPS, ALU.mult, ALU.add, ["st_ss2" + str(pz)], ["st_sd2" + str(pz)])
        tt("pool", rs2, sd2, negh[:, 0:1], ALU.pow, ["st_sd2" + str(pz), "negh"], ["st_rs2" + str(pz)])
        yield
        ob = OUTb[s]
        stt("dve", ob[:, :], h[:, :], rs2, gfin[:, :], ALU.mult, ALU.mult, [hn, "st_rs2" + str(pz), "gfin"], ["OUTb%d" % s])
        dma(out_d[blk * 128:(blk + 1) * 128, :], ob[:, :], ["OUTb%d" % s], ["out_d"], eng="pool")

    exhaust(f_a(0))
    exhaust(f_a(1))
    exhaust(f_a(2))
    for blk in range(32):
        gens = []
        if blk + 3 < 32:
            gens.append((f_a(blk + 3), 1))
        gens.append((f_b(blk), 1))
        drive(*gens)
    P.barrier()

    P.emit(stack)
    stack.close()
    return nc


_NC = None


def _perm32():
    return np.concatenate([np.arange(16, 32), np.arange(0, 16)])


def kernel(x, p, positions, attn_norm_g, w_in, q_norm_g, w_uq, kv_norm_g, w_ukv, conv_w, w_out, moe_norm_g,
           w_group_router, b_group_router, w_expert_router, b_expert_router, w_gate, w_up, w_down, ple_norm_g,
           w_ple_gate, b_ple_gate, w_ple_proj, final_norm_g):
    global _NC
    f = np.float32
    x = np.asarray(x, f)
    p = np.asarray(p, f)
    positions = np.asarray(positions, np.int32)
    w_in0 = np.ascontiguousarray(np.asarray(w_in, f)[0])
    perm = _perm32()
    w_uq0 = np.asarray(w_uq, f)[0]
    w_ukv0 = np.asarray(w_ukv, f)[0]

    def bc(v, n=128):
        return np.ascontiguousarray(np.broadcast_to(np.asarray(v, f).reshape(1, -1), (n, np.asarray(v).size)))

    inv_freq = (10000.0 ** (-np.arange(0, 32, 2, dtype=np.float32) / 32.0)).astype(f)
    rconst = np.zeros((32, 4), f)
    rconst[:, 2] = np.pi / 2
    rconst[:, 0] = np.concatenate([inv_freq, inv_freq])
    rconst[:16, 1] = -1.0
    rconst[16:, 1] = 1.0
    esel = np.zeros((65, 64), f)
    esel[64, :] = 1.0
    capb = np.zeros((128, 64), f)
    capb[:, 0:32] = (np.arange(32) * CAP)[None, :]
    capb[:, 32:64] = ((np.arange(32) + 1) * CAP)[None, :]
    shared = {
        "iota": bc(np.arange(512, dtype=f)),
        "ident": np.eye(128, dtype=f),
        "utri": np.triu(np.ones((128, 128), f), 1),
        "rconst": rconst,
        "esel": esel,
        "capb": capb,
        "g_attn": bc(attn_norm_g[0]),
        "g_moe": bc(moe_norm_g[0]),
        "g_ple": bc(ple_norm_g[0]),
        "g_fin": bc(final_norm_g),
        "b_ple": bc(b_ple_gate[0]),
        "b_rt": bc(np.concatenate([np.asarray(b_group_router, f)[0], np.asarray(b_expert_router, f)[0]])),
        "gq_t": np.ascontiguousarray(np.asarray(q_norm_g, f)[0].reshape(3, 128).T),
        "gkv_t": np.ascontiguousarray(np.asarray(kv_norm_g, f)[0].reshape(2, 128).T),
        "cw_t": np.ascontiguousarray(np.asarray(conv_w, f)[0][:, 0, :].reshape(3, 4, 128).transpose(2, 1, 0).reshape(128, 12)),
        "w_in": w_in0,
        "w_krp": np.ascontiguousarray(w_in0[:, 640 + perm]),
        "w_uqn": np.ascontiguousarray(w_uq0[:, :, 0:64].reshape(QL, NH * 64)),
        "w_uqr": np.ascontiguousarray(w_uq0[:, :, 64:96].reshape(QL, NH * 32)),
        "w_uqp": np.ascontiguousarray(w_uq0[:, :, 64 + perm].reshape(QL, NH * 32)),
        "w_ukn": np.ascontiguousarray(w_ukv0[:, :, 0:64].reshape(KVL, NH * 64)),
        "w_uv": np.ascontiguousarray(w_ukv0[:, :, 64:128].reshape(KVL, NH * 64)),
        "w_out": np.ascontiguousarray(np.asarray(w_out, f)[0]),
        "w_rt": np.ascontiguousarray(np.concatenate([np.asarray(w_group_router, f)[0], np.asarray(w_expert_router, f)[0]], axis=1)),
        "w_gate": np.ascontiguousarray(np.asarray(w_gate, f)[0]),
        "w_up": np.ascontiguousarray(np.asarray(w_up, f)[0]),
        "w_down": np.ascontiguousarray(np.asarray(w_down, f)[0]),
        "w_pg": np.ascontiguousarray(np.asarray(w_ple_gate, f)[0]),
        "w_pp": np.ascontiguousarray(np.asarray(w_ple_proj, f)[0]),
    }
    in_maps = []
    owns = []
    for c in range(8):
        b = c // 2
        hf = c % 2
        T = TILES[hf]
        own = np.concatenate([np.arange(t * 512, (t + 1) * 512) for t in T])
        owns.append(own)
        halo = np.zeros((16, D), f)
        for j, t in enumerate(T):
            if t > 0:
                halo[2 * j:2 * j + 2] = x[b, t * 512 - 2:t * 512]
        kthr = np.zeros((128, 64), f)
        for j, t in enumerate(T):
            delta = t - 2 * j
            for r_ in range(8):
                kthr[:, j * 8 + r_] = r_ * 128 + np.arange(128) - 512 * delta
        m = dict(shared)
        m["x_all"] = np.ascontiguousarray(x[b])
        m["x_own"] = np.ascontiguousarray(x[b][own])
        m["x_halo"] = halo
        m["p_own"] = np.ascontiguousarray(p[0, b][own])
        m["pos_all"] = np.ascontiguousarray(np.broadcast_to(positions[b][None, :], (32, S)))
        m["pos_own"] = np.ascontiguousarray(np.broadcast_to(positions[b][own][None, :], (32, NT)))
        m["kthr"] = kthr
        in_maps.append(m)
    if _NC is None:
        _NC = build_program()
    res = run_bass_kernel_spmd(_NC, in_maps, core_ids=list(range(8)))
    out = np.zeros((4, S, D), f)
    for c in range(8):
        out[c // 2, owns[c]] = np.asarray(res.results[c]["out"], f)
    return out
```

```python
import math
from contextlib import ExitStack
import numpy as np
import concourse.bass as bass
import concourse.mybir as mybir
from concourse.bass_utils import run_bass_kernel_spmd

F32 = mybir.dt.float32
BF16 = mybir.dt.bfloat16
I32 = mybir.dt.int32
AF = mybir.ActivationFunctionType
ALU = mybir.AluOpType
AX = mybir.AxisListType

D = 1024
S = 8192
NT = 4096
QL = 384
KVL = 256
NH = 8
CAP = 384
NE = 32
NSLOT = NE * CAP
EPS = 1e-6
TILES = ([0, 3, 4, 7, 8, 11, 12, 15], [1, 2, 5, 6, 9, 10, 13, 14])
SB_BASE = 16512
SB_END = 229376
PI = math.pi
C1 = 6.28125
C2 = 2.0 * math.pi - 6.28125
SCALE = 96.0 ** -0.5
BIG = 1.0e6


class Op:
    __slots__ = ("eng", "fn", "deps", "alldeps", "dma", "done", "needed", "idx", "amt", "cost", "lat", "bar")


class Prog:
    def __init__(self, nc):
        self.nc = nc
        self.ops = []
        self.lastw = {}
        self.readers = {}
        self.persist_top = SB_BASE
        self.top = SB_BASE
        self.n = 0
        self.last_eng = {}
        self.dma_since = []

    def sb(self, shape, dt, persist=False):
        sz = 1
        for s in shape[1:]:
            sz *= s
        sz *= mybir.dt.size(dt)
        sz = (sz + 63) // 64 * 64
        off = self.top
        self.top += sz
        if persist:
            assert self.persist_top == off, "persist alloc must precede phase allocs"
            self.persist_top = self.top
        assert self.top <= SB_END, ("SBUF overflow", self.top)
        self.n += 1
        return self.nc.alloc_sbuf_tensor_at("t%d" % self.n, list(shape), dt, offset=off)

    def phase_reset(self):
        self.barrier()
        self.top = self.persist_top

    def op(self, eng, fn, r=(), w=(), dma=False, cost=0.5, lat=0.0):
        o = Op()
        o.eng = eng
        o.fn = fn
        o.dma = dma
        o.done = None
        o.needed = dma
        o.idx = len(self.ops)
        o.amt = 16 if dma else 1
        o.cost = cost
        o.lat = lat
        o.bar = False
        deps = set()
        for x in r:
            lw = self.lastw.get(x)
            if lw is not None:
                deps.add((lw, 0))
        for x in w:
            lw = self.lastw.get(x)
            if lw is not None:
                deps.add((lw, 1))
            for rd in self.readers.get(x, ()):
                deps.add((rd, 1))
        need = set()
        alld = set()
        for A, kind in deps:
            if A is o:
                continue
            alld.add(A)
            if A.eng == eng and not A.dma and not dma:
                if eng == "pe":
                    continue
            need.add(A)
        o.deps = need
        o.alldeps = alld
        for x in w:
            self.lastw[x] = o
            self.readers[x] = []
        for x in r:
            if x not in w:
                self.readers.setdefault(x, []).append(o)
        self.ops.append(o)
        return o

    def barrier(self):
        o = Op()
        o.eng = "bar"
        o.fn = None
        o.dma = False
        o.done = None
        o.needed = False
        o.idx = len(self.ops)
        o.amt = 1
        o.cost = 0.0
        o.lat = 0.0
        o.bar = True
        o.deps = set()
        o.alldeps = set()
        self.ops.append(o)

    def schedule(self):
        import heapq
        ENG = ("pe", "act", "dve", "pool", "sp")
        segs = [[]]
        for o in self.ops:
            if o.bar:
                segs.append([])
            else:
                segs[-1].append(o)
        new_ops = []
        for si, seg in enumerate(segs):
            inseg = set(id(o) for o in seg)
            succ = {}
            indeg = {}
            for o in seg:
                d = [a for a in o.alldeps if id(a) in inseg]
                indeg[id(o)] = len(d)
                for a in d:
                    succ.setdefault(id(a), []).append(o)
            bl = {}
            for o in reversed(seg):
                m_ = 0.0
                for c in succ.get(id(o), ()):
                    v_ = bl[id(c)] + 0.3
                    if v_ > m_:
                        m_ = v_
                bl[id(o)] = m_ + o.cost + o.lat
            finish = {}
            efree = {e: 0.0 for e in ENG}
            pend = {e: [] for e in ENG}
            avail = {e: [] for e in ENG}

            def rtime(o):
                t = 0.0
                for a in o.alldeps:
                    f = finish.get(id(a))
                    if f is None:
                        continue
                    f = f + (0.1 if (a.eng == o.eng and not a.dma) else 0.3)
                    if f > t:
                        t = f
                return t

            for o in seg:
                if indeg[id(o)] == 0:
                    heapq.heappush(pend[o.eng], (0.0, -bl[id(o)], o.idx, o))
            order = []
            nleft = len(seg)
            while nleft:
                best = None
                for e in ENG:
                    while pend[e] and pend[e][0][0] <= efree[e]:
                        rt, pr, ix, o = heapq.heappop(pend[e])
                        heapq.heappush(avail[e], (pr, ix, o))
                    if avail[e]:
                        cand = (efree[e], avail[e][0][0], e, 0)
                    elif pend[e]:
                        cand = (pend[e][0][0], pend[e][0][1], e, 1)
                    else:
                        continue
                    if best is None or cand < best:
                        best = cand
                st, ix, e, which = best
                if which == 0:
                    pr, ix, o = heapq.heappop(avail[e])
                else:
                    rt, pr, ix, o = heapq.heappop(pend[e])
                efree[e] = st + (o.cost if not o.dma else (0.2 if e == "sp" else 1.2))
                finish[id(o)] = st + o.cost + o.lat
                order.append(o)
                nleft -= 1
                for c in succ.get(id(o), ()):
                    indeg[id(c)] -= 1
                    if indeg[id(c)] == 0:
                        heapq.heappush(pend[c.eng], (rtime(c), -bl[id(c)], c.idx, c))
            new_ops.extend(order)
            if si < len(segs) - 1 or True:
                lasts = {}
                dmas = []
                for o in order:
                    if o.dma:
                        dmas.append(o)
                    else:
                        lasts[o.eng] = o
                for e in ENG:
                    b = Op()
                    b.eng = e
                    b.fn = None
                    b.dma = False
                    b.done = None
                    b.needed = False
                    b.idx = -1
                    b.amt = 1
                    b.cost = 0.0
                    b.lat = 0.0
                    b.bar = False
                    b.deps = set(dmas) | set(v for k, v in lasts.items() if k != e)
                    b.alldeps = set()
                    for a in b.deps:
                        a.needed = True
                    new_ops.append(b)
        for o in new_ops:
            for a in o.deps:
                a.needed = True
        self.ops = new_ops

    def emit(self, stack):
        nc = self.nc
        EPOCH = 3000
        NDS = 12
        comp = {}
        sems = {}

        def getsem(key):
            if key not in sems:
                sems[key] = stack.enter_context(nc.semaphore("s_%s_%s" % key))
            return sems[key]

        self.schedule()
        cnt = {}
        dcnt = {}
        dval = {}
        dlast = {}
        for o in self.ops:
            if o.dma:
                k = dcnt.get(o.eng, 0)
                dcnt[o.eng] = k + 1
                key = ("d" + o.eng, k % NDS)
                prev = dlast.get(key)
                if prev is not None:
                    o.deps.add(prev)
                dlast[key] = o
                v = dval.get(key, 0) + 16
                dval[key] = v
                o.done = (getsem(key), v)
            elif o.needed:
                c = cnt.get(o.eng, 0)
                cnt[o.eng] = c + 1
                key = (o.eng, c // EPOCH)
                o.done = (getsem(key), c % EPOCH + 1)
        per = {e: [] for e in ("pe", "act", "dve", "pool", "sp")}
        for o in self.ops:
            per[o.eng].append(o)
        ops = self.ops
        engs = {"pe": "tensor", "act": "scalar", "dve": "vector", "pool": "gpsimd", "sp": "sync"}

        def replay(name):
            def run(e):
                waited = {}
                if name == "pool":
                    reg = e.alloc_register("bchk")
                    e.reg_mov(reg, NSLOT - 1)
                    self.breg = reg
                for o in per[name]:
                    ws = {}
                    for a in o.deps:
                        sem, val = a.done
                        k = id(sem)
                        if k not in ws or ws[k][1] < val:
                            ws[k] = (sem, val)
                    for k, (sem, val) in ws.items():
                        if waited.get(k, 0) >= val:
                            continue
                        waited[k] = val
                        e.wait_ge(sem, val)
                    if o.fn is not None:
                        ins = o.fn(e)
                        if o.done is not None:
                            ins.then_inc(o.done[0], o.amt)
            return run

        with nc.Block() as block:
            for name, attr in engs.items():
                getattr(block, attr)(replay(name))


def build_program():
    nc = bass.Bass("TRN2", target_bir_lowering=False)
    P = Prog(nc)
    stack = ExitStack()

    def din(name, shape, dt=F32):
        return nc.dram_tensor(name, list(shape), dt, kind="ExternalInput").ap()

    def dscr(name, shape, dt):
        return nc.dram_tensor(name, list(shape), dt, kind="Internal").ap()

    x_all = din("x_all", [S, D])
    x_own = din("x_own", [NT, D])
    x_halo = din("x_halo", [16, D])
    p_own = din("p_own", [NT, 256])
    pos_all = din("pos_all", [32, S], I32)
    pos_own = din("pos_own", [32, NT], I32)
    kthr_d = din("kthr", [128, 64])
    iota_d = din("iota", [128, 512])
    ident_d = din("ident", [128, 128])
    utri_d = din("utri", [128, 128])
    rconst_d = din("rconst", [32, 4])
    esel_d = din("esel", [65, 64])
    capb_d = din("capb", [128, 64])
    g_attn_d = din("g_attn", [128, D])
    g_moe_d = din("g_moe", [128, D])
    g_ple_d = din("g_ple", [128, D])
    g_fin_d = din("g_fin", [128, D])
    b_ple_d = din("b_ple", [128, D])
    b_rt_d = din("b_rt", [128, 36])
    gq_d = din("gq_t", [128, 3])
    gkv_d = din("gkv_t", [128, 2])
    cw_d = din("cw_t", [128, 12])
    w_in_d = din("w_in", [D, 2208])
    w_krp_d = din("w_krp", [D, 32])
    w_uqn_d = din("w_uqn", [QL, NH * 64])
    w_uqr_d = din("w_uqr", [QL, NH * 32])
    w_uqp_d = din("w_uqp", [QL, NH * 32])
    w_ukn_d = din("w_ukn", [KVL, NH * 64])
    w_uv_d = din("w_uv", [KVL, NH * 64])
    w_out_d = din("w_out", [D, D])
    w_rt_d = din("w_rt", [D, 36])
    w_gate_d = din("w_gate", [NE, D, 512])
    w_up_d = din("w_up", [NE, D, 512])
    w_down_d = din("w_down", [NE, 512, D])
    w_pg_d = din("w_pg", [D, D])
    w_pp_d = din("w_pp", [256, D])
    out_d = nc.dram_tensor("out", [NT, D], F32, kind="ExternalOutput").ap()

    ocv_s = dscr("ocv_s", [4, 128, NT], BF16)
    om_s = dscr("om_s", [NH, 64, NT], BF16)
    h1_s = dscr("h1_s", [NT, D], F32)
    xe_s = dscr("xe_s", [NSLOT, D], BF16)
    ye_s = dscr("ye_s", [NSLOT, D], F32)

    PS = [stack.enter_context(nc.psum_tensor("ps%d" % i, [128, 512], F32)) for i in range(8)]

    def psb(i):
        return PS[i][:, :].bitcast(BF16)

    def fsz(ap):
        n = 1
        for d_ in ap.shape[1:]:
            n *= int(d_)
        return n

    def dma(out, in_, r, w, eng="sp"):
        nbytes = fsz(out) * int(out.shape[0]) * mybir.dt.size(out.dtype)
        lat = 2.0 + nbytes / 150e3
        if eng == "sp":
            P.op("sp", lambda e: e.dma_start(out=out, in_=in_), r, w, dma=True, cost=0.2, lat=lat)
        else:
            P.op("pool", lambda e: e.dma_start(out=out, in_=in_), r, w, dma=True, cost=1.2, lat=lat + 1.0)

    def mm(out, lhsT, rhs, st, sp_, r, w):
        c = max(fsz(rhs), 64) / 1900.0 + 0.03
        if rhs.dtype == F32:
            c *= 4
        P.op("pe", lambda e: e.matmul(out, lhsT, rhs, start=st, stop=sp_), r, w, cost=c)

    def tr(out, in_, ident, r, w):
        c = 0.3 if in_.dtype == F32 else 0.1
        P.op("pe", lambda e: e.transpose(out, in_, ident), r, w, cost=c)

    def act(out, in_, func, r, w, bias=0.0, scale=1.0, accum=None):
        c = 0.25 + fsz(in_) / 1300.0
        if accum is None:
            P.op("act", lambda e: e.activation(out, in_, func, bias=bias, scale=scale), r, w, cost=c)
        else:
            P.op("act", lambda e: e.activation(out, in_, func, bias=bias, scale=scale, accum_out=accum), r, w, cost=c + 0.15)

    def vcost(eng, n):
        if eng == "pool":
            return 1.0 + n / 250.0
        return 0.12 + n / 900.0

    def ts(eng, out, in0, s1, s2, op0, op1, r, w):
        c = vcost(eng, fsz(in0))
        if op1 is None:
            P.op(eng, lambda e: e.tensor_scalar(out, in0, s1, None, op0), r, w, cost=c)
        else:
            P.op(eng, lambda e: e.tensor_scalar(out, in0, s1, s2, op0, op1), r, w, cost=c)

    def tt(eng, out, in0, in1, op, r, w):
        P.op(eng, lambda e: e.tensor_tensor(out, in0, in1, op), r, w, cost=vcost(eng, fsz(in0)))

    def stt(eng, out, in0, sc, in1, op0, op1, r, w):
        P.op(eng, lambda e: e.scalar_tensor_tensor(out, in0, sc, in1, op0, op1), r, w, cost=vcost(eng, fsz(in0)))

    def cp(eng, out, in_, r, w):
        if eng == "act":
            P.op("act", lambda e: e.copy(out, in_), r, w, cost=0.25 + fsz(in_) / 1300.0)
        else:
            P.op(eng, lambda e: e.tensor_copy(out, in_), r, w, cost=vcost(eng, fsz(in_)))

    def recip(out, in_, r, w):
        P.op("dve", lambda e: e.reciprocal(out, in_), r, w, cost=vcost("dve", fsz(in_)))

    def memset(eng, ap, val, w):
        P.op(eng, lambda e: e.memset(ap, val), (), w, cost=vcost(eng, fsz(ap)))

    def redmax(out, in_, r, w):
        P.op("dve", lambda e: e.reduce_max(out, in_, AX.X), r, w, cost=vcost("dve", fsz(in_)))

    def redsum(out, in_, r, w):
        P.op("dve", lambda e: e.reduce_sum(out, in_, AX.X), r, w, cost=vcost("dve", fsz(in_)))

    def drive(*pairs):
        live = [[g, n] for g, n in pairs]
        while live:
            for it in list(live):
                g, n = it
                for _ in range(n):
                    try:
                        next(g)
                    except StopIteration:
                        live.remove(it)
                        break

    def exhaust(g):
        for _ in g:
            pass

    ident_f = P.sb([128, 128], F32, True)
    ident_b = P.sb([128, 128], BF16, True)
    utri_b = P.sb([128, 128], BF16, True)
    ones_b = P.sb([128, 128], BF16, True)
    rconst = P.sb([32, 4], F32, True)
    stats = P.sb([128, 16], F32, True)
    tmpc = P.sb([128, 128], F32, True)
    epsc = P.sb([128, 1], F32, True)

    dma(ident_f[:, :], ident_d, (), ["ident_f"])
    cp("dve", ident_b[:, :], ident_f[:, :], ["ident_f"], ["ident_b"])
    dma(tmpc[:, :], utri_d, (), ["tmpc"])
    cp("dve", utri_b[:, :], tmpc[:, :], ["tmpc"], ["utri_b"])
    memset("pool", ones_b[:, :], 1.0, ["ones_b"])
    memset("pool", epsc[:, :], EPS, ["epsc"])
    dma(rconst[:, :], rconst_d, (), ["rconst"])

    def wchunks(dram2d, c0, c1):
        return dram2d[:, c0:c1].rearrange("(k p) c -> p k c", p=128)

    def norm_block(tag, xb, nrows, gbc, xn, sx=0):
        c0 = 4 * sx
        q = str(sx)
        ss = stats[0:nrows, c0:c0 + 1]
        sd = stats[0:nrows, c0 + 1:c0 + 2]
        rs = stats[0:nrows, c0 + 2:c0 + 3]
        act(junk[0:nrows, :], xb, AF.Square, [tag + "xb"], ["junk", "st_ss" + q], accum=ss)
        act(sd, ss, AF.Ln, ["st_ss" + q, "epsc"], ["st_sd" + q], bias=epsc[0:nrows, 0:1], scale=1.0 / D)
        act(rs, sd, AF.Exp, ["st_sd" + q], ["st_rs" + q], scale=-0.5)
        stt("dve", xn, xb, rs, gbc[0:nrows, :], ALU.mult, ALU.mult, [tag + "xb", "st_rs" + q, "gbc"], [tag + "xn"])

    def transposes_bf(tag, xn, nrows, psi, dst, nk=8):
        pv = psb(psi)
        for k in range(nk):
            tr(pv[:, k * nrows:(k + 1) * nrows], xn[:, k * 128:(k + 1) * 128], ident_b[0:nrows, 0:nrows],
               [tag + "xn", "ident_b"], ["ps%d" % psi])
        return pv[:, 0:nk * nrows].rearrange("p (k t) -> p k t", k=nk)

    def rope_tables(tag, pos_i, n, cos_o, sin_o, tmp):
        pf, ang, a2, kq, kf, r1, wv = tmp
        cp("dve", pf[:, 0:n], pos_i, [tag + "pos"], [tag + "pf"])
        ts("dve", ang[:, 0:n], pf[:, 0:n], rconst[:, 0:1], None, ALU.mult, None, [tag + "pf", "rconst"], [tag + "ang"])
        ts("dve", kq[:, 0:n], ang[:, 0:n], 1.0 / (2 * PI), None, ALU.mult, None, [tag + "ang"], [tag + "kq"])
        cp("dve", kf[:, 0:n], kq[:, 0:n], [tag + "kq"], [tag + "kf"])
        stt("dve", r1[:, 0:n], kf[:, 0:n], -C1, ang[:, 0:n], ALU.mult, ALU.add, [tag + "kf", tag + "ang"], [tag + "r1"])
        stt("dve", r1[:, 0:n], kf[:, 0:n], -C2, r1[:, 0:n], ALU.mult, ALU.add, [tag + "kf", tag + "r1"], [tag + "r1"])
        ts("dve", wv[:, 0:n], r1[:, 0:n], PI, -2 * PI, ALU.is_gt, ALU.mult, [tag + "r1"], [tag + "wv"])
        tt("dve", r1[:, 0:n], r1[:, 0:n], wv[:, 0:n], ALU.add, [tag + "r1", tag + "wv"], [tag + "r1"])
        ts("dve", r1[:, 0:n], r1[:, 0:n], PI, -PI, ALU.min, ALU.max, [tag + "r1"], [tag + "r1"])
        act(sin_o, r1[:, 0:n], AF.Sin, [tag + "r1", "rconst"], [tag + "sin"], scale=rconst[:, 1:2])
        stt("dve", a2[:, 0:n], r1[:, 0:n], -1.0, r1[:, 0:n], ALU.mult, ALU.max, [tag + "r1"], [tag + "a2"])
        act(cos_o, a2[:, 0:n], AF.Sin, [tag + "a2", "rconst"], [tag + "cos"], bias=rconst[:, 2:3], scale=-1.0)

    const_top = P.persist_top
    CKV = P.sb([128, 2, S], BF16, True)
    CQ = P.sb([128, 3, NT], BF16, True)
    KT = [P.sb([96, S], BF16, True) for _ in range(2)]
    COSQ = P.sb([32, NT], F32, True)
    SINQ = P.sb([32, NT], F32, True)
    persist_mark = P.persist_top

    junk = P.sb([128, D], BF16)
    gbc = P.sb([128, D], F32)
    XB = [P.sb([128, D], F32) for _ in range(3)]
    XN = [P.sb([128, D], BF16) for _ in range(3)]
    XT = [P.sb([128, 8, 512], BF16) for _ in range(2)]
    WCV = P.sb([128, 8, 1536], BF16)
    cw = P.sb([128, 12], F32)
    XTh = P.sb([128, 8, 16], BF16)
    CUH = P.sb([128, 4, 16], F32)
    usb = P.sb([128, 512], F32)
    CU = [P.sb([128, 514], F32) for _ in range(2)]
    acc = P.sb([128, 512], F32)
    OCb = [P.sb([128, 512], BF16) for _ in range(2)]

    dma(gbc[:, :], g_attn_d, (), ["gbc"])
    dma(cw[:, :], cw_d, (), ["cw"])
    dma(WCV[:, :, :], wchunks(w_in_d, 672, 2208), (), ["WCV"], eng="pool")
    dma(XB[0][0:16, :], x_halo, (), ["X0xb"])
    norm_block("X0", XB[0][0:16, :], 16, gbc, XN[0][0:16, :])
    pv = transposes_bf("X0", XN[0][0:16, :], 16, 0, None)
    cp("dve", XTh[:, :, :], pv, ["ps0"], ["XTh"])
    for cc in range(4):
        for k in range(8):
            mm(PS[1][:, cc * 16:(cc + 1) * 16], WCV[:, k, 512 + cc * 128:512 + (cc + 1) * 128], XTh[:, k, :],
               k == 0, k == 7, ["WCV", "XTh"], ["ps1"])
        for k in range(8):
            mm(PS[2][:, cc * 16:(cc + 1) * 16], WCV[:, k, 1024 + cc * 128:1024 + (cc + 1) * 128], XTh[:, k, :],
               k == 0, k == 7, ["WCV", "XTh"], ["ps2"])
    cp("act", usb[:, 0:64], PS[2][:, 0:64], ["ps2"], ["usb"])
    tt("dve", CUH[:, :, :].rearrange("p a b -> p (a b)"), PS[1][:, 0:64], usb[:, 0:64], ALU.mult, ["ps1", "usb"], ["CUH"])

    def own_tile_norm(j, gname_unused, xt, tagp, src):
        for bi in range(4):
            blk = j * 4 + bi
            s = blk % 2
            sx = blk % 3
            tg = "X%d" % sx
            dma(XB[sx][:, :], src[blk * 128:(blk + 1) * 128, :], (), [tg + "xb"])
            norm_block(tg, XB[sx][:, :], 128, gbc, XN[sx][:, :], sx)
            pv_ = transposes_bf(tg, XN[sx][:, :], 128, s, None)
            cp("act" if bi % 2 else "dve", xt[:, :, bi * 128:(bi + 1) * 128], pv_, ["ps%d" % s], ["XT%d" % (j % 2)])
            yield

    def bc_tail(j):
        xt = XT[j % 2]
        xtn = "XT%d" % (j % 2)
        for cc in range(4):
            pb0 = 2 + 3 * (cc % 2)
            for gi, base in enumerate((0, 512, 1024)):
                for k in range(8):
                    mm(PS[pb0 + gi][:, :], WCV[:, k, base + cc * 128:base + (cc + 1) * 128], xt[:, k, :],
                       k == 0, k == 7, ["WCV", xtn], ["ps%d" % (pb0 + gi)])
            cu = CU[cc % 2]
            cun = "CU%d" % (cc % 2)
            us_ = usb2[cc % 2]
            usn = "usb%d" % (cc % 2)
            ac_ = acc2[cc % 2]
            acn = "acc%d" % (cc % 2)
            cp("act", us_[:, :], PS[pb0 + 2][:, :], ["ps%d" % (pb0 + 2)], [usn])
            tt("dve", cu[:, 2:514], PS[pb0 + 1][:, :], us_[:, :], ALU.mult, ["ps%d" % (pb0 + 1), usn], [cun])
            cp("dve", cu[:, 0:2], CUH[:, cc, 2 * j:2 * j + 2], ["CUH"], [cun + "h"])
            ts("dve", ac_[:, :], cu[:, 2:514], cw[:, cc * 3 + 2:cc * 3 + 3], None, ALU.mult, None, [cun, "cw"], [acn])
            stt("dve", ac_[:, :], cu[:, 1:513], cw[:, cc * 3 + 1:cc * 3 + 2], ac_[:, :], ALU.mult, ALU.add,
                [cun, cun + "h", "cw", acn], [acn])
            stt("dve", ac_[:, :], cu[:, 0:512], cw[:, cc * 3:cc * 3 + 1], ac_[:, :], ALU.mult, ALU.add,
                [cun, cun + "h", "cw", acn], [acn])
            ob = OCb[cc % 2]
            tt("dve", ob[:, :], ac_[:, :], PS[pb0][:, :], ALU.mult, [acn, "ps%d" % pb0], ["OCb%d" % (cc % 2)])
            dma(ocv_s[cc, :, j * 512:(j + 1) * 512], ob[:, :], ["OCb%d" % (cc % 2)], ["ocv_s"], eng="pool")
            yield

    usb2 = [usb, P.sb([128, 512], F32)]
    acc2 = [acc, P.sb([128, 512], F32)]
    exhaust(own_tile_norm(0, None, XT[0], "B", x_own))
    for j in range(8):
        gens = []
        if j + 1 < 8:
            gens.append((own_tile_norm(j + 1, None, XT[(j + 1) % 2], "B", x_own), 1))
        gens.append((bc_tail(j), 1))
        drive(*gens)

    P.phase_reset()
    junk = P.sb([128, D], BF16)
    gbc = P.sb([128, D], F32)
    XB = [P.sb([128, D], F32) for _ in range(3)]
    XN = [P.sb([128, D], BF16) for _ in range(3)]
    XT = [P.sb([128, 8, 512], BF16) for _ in range(2)]
    WKV = P.sb([128, 8, 256], BF16)
    WKR = P.sb([128, 8, 32], BF16)
    WKP = P.sb([128, 8, 32], BF16)
    WQI = P.sb([128, 8, QL], BF16)
    gkv = P.sb([128, 2], F32)
    gq = P.sb([128, 3], F32)
    SQ = P.sb([128, 3, 512], BF16)
    STD = P.sb([128, 512], F32)
    RS = P.sb([128, 512], F32)
    posi = P.sb([32, 512], I32)
    rtmp = [P.sb([32, 512], F32) for _ in range(3)] + [P.sb([32, 512], I32)] + [P.sb([32, 512], F32) for _ in range(3)]
    cosk = P.sb([32, 512], F32)
    sink = P.sb([32, 512], F32)
    t1 = P.sb([32, 512], F32)
    t2 = P.sb([32, 512], F32)

    dma(gbc[:, :], g_attn_d, (), ["gbc"])
    dma(gkv[:, :], gkv_d, (), ["gkv"])
    dma(gq[:, :], gq_d, (), ["gq0"])
    ts("dve", gq[:, :], gq[:, :], SCALE, None, ALU.mult, None, ["gq0"], ["gq"])
    dma(WKV[:, :, :], wchunks(w_in_d, 384, 640), (), ["WKV"], eng="pool")
    dma(WKR[:, :, :], wchunks(w_in_d, 640, 672), (), ["WKR"], eng="pool")
    dma(WKP[:, :, :], wchunks(w_krp_d, 0, 32), (), ["WKP"], eng="pool")
    dma(WQI[:, :, :], wchunks(w_in_d, 0, 384), (), ["WQI"], eng="pool")

    def a_tail(t):
        xt = XT[t % 2]
        xtn = "XT%d" % (t % 2)
        for c in range(2):
            for k in range(8):
                mm(PS[2 + c][:, :], WKV[:, k, c * 128:(c + 1) * 128], xt[:, k, :], k == 0, k == 7, ["WKV", xtn], ["ps%d" % (2 + c)])
        for k in range(8):
            mm(PS[4][0:32, :], WKR[:, k, :], xt[:, k, :], k == 0, k == 7, ["WKR", xtn], ["ps4"])
        for k in range(8):
            mm(PS[5][0:32, :], WKP[:, k, :], xt[:, k, :], k == 0, k == 7, ["WKP", xtn], ["ps5"])
        for c in range(2):
            act(SQ[:, c, :], PS[2 + c][:, :], AF.Square, ["ps%d" % (2 + c)], ["SQ%d" % c])
        for c in range(2):
            mm(PS[6][:, :], ones_b[:, :], SQ[:, c, :], c == 0, c == 1, ["ones_b", "SQ%d" % c], ["ps6"])
        act(STD[:, :], PS[6][:, :], AF.Ln, ["ps6", "epsc"], ["STD"], bias=epsc[:, 0:1], scale=1.0 / KVL)
        yield
        act(RS[:, :], STD[:, :], AF.Exp, ["STD"], ["RS"], scale=-0.5)
        yield
        for c in range(2):
            stt("dve", CKV[:, c, t * 512:(t + 1) * 512], PS[2 + c][:, :], gkv[:, c:c + 1], RS[:, :], ALU.mult, ALU.mult,
                ["ps%d" % (2 + c), "gkv", "RS"], ["CKV"])
        dma(posi[:, :], pos_all[:, t * 512:(t + 1) * 512], (), ["Kpos"])
        yield
        rope_tables("K", posi[:, :], 512, cosk[:, :], sink[:, :], rtmp)
        yield
        tt("dve", t1[:, :], PS[4][0:32, :], cosk[:, :], ALU.mult, ["ps4", "Kcos"], ["t1"])
        yield
        tt("dve", t2[:, :], PS[5][0:32, :], sink[:, :], ALU.mult, ["ps5", "Ksin"], ["t2"])
        yield
        tt("pool", KT[0][0:32, t * 512:(t + 1) * 512], t1[:, :], t2[:, :], ALU.add, ["t1", "t2"], ["KT0r"])
        yield
        tt("pool", KT[1][0:32, t * 512:(t + 1) * 512], t1[:, :], t2[:, :], ALU.add, ["t1", "t2"], ["KT1r"])
        yield

    def q_tail(j):
        xt = XT[j % 2]
        xtn = "XT%d" % (j % 2)
        for c in range(3):
            for k in range(8):
                mm(PS[2 + c][:, :], WQI[:, k, c * 128:(c + 1) * 128], xt[:, k, :], k == 0, k == 7, ["WQI", xtn], ["ps%d" % (2 + c)])
        for c in range(3):
            act(SQ[:, c, :], PS[2 + c][:, :], AF.Square, ["ps%d" % (2 + c)], ["SQ%d" % c])
        for c in range(3):
            mm(PS[6][:, :], ones_b[:, :], SQ[:, c, :], c == 0, c == 2, ["ones_b", "SQ%d" % c], ["ps6"])
        act(STD[:, :], PS[6][:, :], AF.Ln, ["ps6", "epsc"], ["STD"], bias=epsc[:, 0:1], scale=1.0 / QL)
        yield
        act(RS[:, :], STD[:, :], AF.Exp, ["STD"], ["RS"], scale=-0.5)
        yield
        for c in range(3):
            stt("dve", CQ[:, c, j * 512:(j + 1) * 512], PS[2 + c][:, :], gq[:, c:c + 1], RS[:, :], ALU.mult, ALU.mult,
                ["ps%d" % (2 + c), "gq", "RS"], ["CQ"])
        dma(posi[:, :], pos_own[:, j * 512:(j + 1) * 512], (), ["Kpos"])
        yield
        rope_tables("K", posi[:, :], 512, COSQ[:, j * 512:(j + 1) * 512], SINQ[:, j * 512:(j + 1) * 512], rtmp)
        yield

    fronts = [(t, x_all) for t in range(16)] + [(j, x_own) for j in range(8)]
    exhaust(own_tile_norm(fronts[0][0], None, XT[0], "A", fronts[0][1]))
    for i_ in range(24):
        gens = []
        if i_ + 1 < 24:
            gens.append((own_tile_norm(fronts[i_ + 1][0], None, XT[(i_ + 1) % 2], "A", fronts[i_ + 1][1]), 1))
        if i_ < 16:
            gens.append((a_tail(i_), 3))
        else:
            gens.append((q_tail(i_ - 16), 3))
        drive(*gens)

    P.phase_reset()
    Vb = [P.sb([128, 64, 65], BF16) for _ in range(2)]
    QT = [P.sb([96, NT], BF16) for _ in range(2)]
    WQn = P.sb([128, 3, NH, 96], BF16)
    WQr = P.sb([128, 3, NH, 32], BF16)
    WQp = P.sb([128, 3, NH, 32], BF16)
    WKn = P.sb([128, 2, NH, 96], BF16)
    WV = P.sb([128, 2, NH, 64], BF16)
    PT = [P.sb([128, 512], BF16) for _ in range(3)]
    Osb = [P.sb([65, 512], F32) for _ in range(2)]
    rden = P.sb([64, 512], F32)
    OMb = [P.sb([64, 512], BF16) for _ in range(2)]
    qt1 = P.sb([32, 512], F32)
    qt2 = P.sb([32, 512], F32)
    kthr = P.sb([128, 64], F32)
    iota = P.sb([128, 512], F32)
    esel = P.sb([65, 64], F32)

    dma(kthr[:, :], kthr_d, (), ["kthr"])
    dma(iota[:, :], iota_d, (), ["iota"])
    dma(esel[:, :], esel_d, (), ["esel"])
    memset("pool", WQn[:, :, :, :], 0.0, ["WQn"])
    memset("pool", WKn[:, :, :, :], 0.0, ["WKn"])
    for b_ in range(2):
        memset("pool", Vb[b_][:, :, 64:65], 1.0, ["Vb%done" % b_])
    def dma_w4(dst, src2d, nch, lo, hi, name):
        for c_ in range(nch):
            dma(dst[:, c_, :, lo:hi], src2d[c_ * 128:(c_ + 1) * 128, :].rearrange("p (h d) -> p h d", h=NH), (), [name], eng="pool")

    dma_w4(WQn, w_uqn_d, 3, 32, 96, "WQn")
    dma_w4(WQr, w_uqr_d, 3, 0, 32, "WQr")
    dma_w4(WQp, w_uqp_d, 3, 0, 32, "WQp")
    dma_w4(WKn, w_ukn_d, 2, 32, 96, "WKn")
    dma_w4(WV, w_uv_d, 2, 0, 64, "WV")

    def gen_units(h):
        hb = h % 2
        kb_, vb_, qb_ = KT[hb], Vb[hb], QT[hb]
        kn, vn, qn = "KT%dn" % hb, "Vb%d" % hb, "QT%d" % hb
        units = []

        def ku(t):
            for c in range(2):
                mm(PS[5][0:96, :], WKn[:, c, h, :], CKV[:, c, t * 512:(t + 1) * 512], c == 0, c == 1, ["WKn", "CKV"], ["ps5"])
            cp("dve", kb_[32:64, t * 512:(t + 1) * 512], PS[5][32:64, :], ["ps5"], [kn + "a"])
            cp("dve", kb_[64:96, t * 512:(t + 1) * 512], PS[5][64:96, :], ["ps5"], [kn + "b"])

        def vu(g8):
            for i8 in range(8):
                blk = g8 * 8 + i8
                for c in range(2):
                    mm(PS[6][:, i8 * 64:(i8 + 1) * 64], CKV[:, c, blk * 128:(blk + 1) * 128], WV[:, c, h, :], c == 0, c == 1,
                       ["CKV", "WV"], ["ps6"])
            cp("dve", vb_[:, g8 * 8:(g8 + 1) * 8, 0:64], PS[6][:, :].rearrange("p (a b) -> p a b", a=8), ["ps6"], [vn])

        def qu(j):
            sl = slice(j * 512, (j + 1) * 512)
            for c in range(3):
                mm(PS[5][0:96, :], WQn[:, c, h, :], CQ[:, c, sl], c == 0, c == 2, ["WQn", "CQ"], ["ps5"])
            for c in range(3):
                mm(PS[6][0:32, :], WQr[:, c, h, :], CQ[:, c, sl], c == 0, c == 2, ["WQr", "CQ"], ["ps6"])
            for c in range(3):
                mm(PS[7][0:32, :], WQp[:, c, h, :], CQ[:, c, sl], c == 0, c == 2, ["WQp", "CQ"], ["ps7"])
            cp("dve", qb_[32:64, sl], PS[5][32:64, :], ["ps5"], [qn + "a"])
            cp("dve", qb_[64:96, sl], PS[5][64:96, :], ["ps5"], [qn + "b"])
            tt("dve", qt1[:, :], PS[6][0:32, :], COSQ[:, sl], ALU.mult, ["ps6", "Kcos"], ["qt1"])
            tt("dve", qt2[:, :], PS[7][0:32, :], SINQ[:, sl], ALU.mult, ["ps7", "Ksin"], ["qt2"])
            tt("pool", qb_[0:32, sl], qt1[:, :], qt2[:, :], ALU.add, ["qt1", "qt2"], [qn + "r"])

        for t in range(16):
            units.append(lambda t=t: ku(t))
        for g8 in range(8):
            units.append(lambda g8=g8: vu(g8))
        for j in range(8):
            units.append(lambda j=j: qu(j))
        return units

    for u in gen_units(0):
        u()
    cnt_sl = 0
    LA = 2
    for h in range(NH):
        hb = h % 2
        kb_, vb_, qb_ = KT[hb], Vb[hb], QT[hb]
        kn, vn, qn = "KT%dn" % hb, "Vb%d" % hb, "QT%d" % hb
        items = [(j, kb) for j in range(8) for kb in range(8 * (j + 1))]
        n_it = len(items)
        nxt = gen_units(h + 1) if h + 1 < NH else []
        deferred = {}
        slot_ob = {}
        for i in range(n_it + LA + 4):
            if i < n_it:
                j, kb = items[i]
                sl = slice(j * 512, (j + 1) * 512)
                sb_ = i % 3
                mm(PS[sb_][:, :], kb_[0:96, kb * 128:(kb + 1) * 128], qb_[0:96, sl], True, True,
                   [kn + "a", kn + "b", "KT%dr" % hb, qn + "a", qn + "b", qn + "r"], ["ps%d" % sb_])
                pt = PT[sb_]
                ptn = "PT%d" % sb_
                act(pt[:, :], PS[sb_][:, :], AF.Exp, ["ps%d" % sb_], [ptn])
                if kb >= 8 * j:
                    col = j * 8 + (kb - 8 * j)
                    stt("dve", pt[:, :], iota[:, :], kthr[:, col:col + 1], pt[:, :], ALU.is_ge, ALU.mult,
                        ["iota", "kthr", ptn], [ptn])
            if LA <= i < n_it + LA:
                j, kb = items[i - LA]
                sl = slice(j * 512, (j + 1) * 512)
                sb_ = (i - LA) % 3
                pt = PT[sb_]
                ptn = "PT%d" % sb_
                if kb == 0:
                    slot_ob[j] = cnt_sl
                    cnt_sl += 1
                cs_ = slot_ob[j]
                ob = 3 + (cs_ % 2)
                obn = "ps%d" % ob
                nkb = 8 * (j + 1)
                mm(PS[ob][0:65, :], vb_[:, kb, :], pt[:, :], kb == 0, kb == nkb - 1, [vn, vn + "one", ptn], [obn])
                if kb == nkb - 1:
                    os_ = Osb[cs_ % 2]
                    osn = "Osb%d" % (cs_ % 2)
                    cp("dve", os_[:, :], PS[ob][0:65, :], [obn], [osn])

                    def fin(os_=os_, osn=osn, cs_=cs_, sl=sl, h=h):
                        mm(PS[7][0:64, :], esel[:, :], os_[:, :], True, True, ["esel", osn], ["ps7"])
                        recip(rden[:, :], PS[7][0:64, :], ["ps7"], ["rden"])
                        omb = OMb[cs_ % 2]
                        tt("pool", omb[:, :], os_[0:64, :], rden[:, :], ALU.mult, [osn, "rden"], ["OMb%d" % (cs_ % 2)])
                        dma(om_s[h, :, sl], omb[:, :], ["OMb%d" % (cs_ % 2)], ["om_s"], eng="pool")
                    deferred.setdefault(i + 4, []).append(fin)
            for fn_ in deferred.pop(i, []):
                fn_()
            if nxt and i >= 8 and i % 8 == 0:
                nxt.pop(0)()
        for k_ in sorted(deferred):
            for fn_ in deferred[k_]:
                fn_()
        while nxt:
            nxt.pop(0)()

    P.barrier()
    P.persist_top = const_top
    P.top = const_top
    SL = P.sb([128, 32, 2], I32, True)
    WTS = P.sb([128, 32, 2], F32, True)
    CB = P.sb([128, 32], F32, True)
    LIM = P.sb([128, 32], F32, True)
    persist_rt = P.persist_top
    SG = [P.sb([128, 8, 512], F32, True) for _ in range(2)]
    SU = [P.sb([128, 8, 512], F32, True) for _ in range(2)]
    SD = [P.sb([128, 4, D], F32, True) for _ in range(2)]
    persist2 = P.persist_top

    def wload(e_):
        eb = e_ % 2
        dma(SG[eb][:, :, :], w_gate_d[e_].rearrange("(k p) f -> p k f", p=128), (), ["SG%d" % eb])
        dma(SU[eb][:, :, :], w_up_d[e_].rearrange("(k p) f -> p k f", p=128), (), ["SU%d" % eb])
        dma(SD[eb][:, :, :], w_down_d[e_].rearrange("(k p) f -> p k f", p=128), (), ["SD%d" % eb])

    wload(0)
    wload(1)

    junk = P.sb([128, D], BF16)
    gbc = P.sb([128, D], F32)
    XB = [P.sb([128, D], F32) for _ in range(2)]
    WOm = P.sb([128, 4, D], BF16)
    WOc = P.sb([128, 4, D], BF16)
    WR = P.sb([128, 8, 36], F32)
    brt = P.sb([128, 36], F32)
    OMs = [P.sb([128, 4, 512], BF16) for _ in range(2)]
    OCs = [P.sb([128, 4, 512], BF16) for _ in range(2)]
    H1b = [P.sb([128, D], F32) for _ in range(2)]
    XNFb = [P.sb([128, D], F32) for _ in range(2)]
    XN2 = [P.sb([128, D], BF16) for _ in range(4)]
    XNFTb = [P.sb([128, 8, 128], F32) for _ in range(2)]
    Rb = [P.sb([128, 256], F32) for _ in range(2)]
    A32b = [P.sb([128, 32], BF16) for _ in range(2)]
    sli = [P.sb([128, 2], I32) for _ in range(4)]

    dma(gbc[:, :], g_moe_d, (), ["gbc"])
    dma(brt[:, :], b_rt_d, (), ["brt"])
    dma(WR[:, :, :], wchunks(w_rt_d, 0, 36), (), ["WR"])
    dma(WOm[:, :, :], w_out_d[0:512, :].rearrange("(h p) c -> p h c", p=128), (), ["WOm"], eng="pool")
    dma(WOc[:, :, :], w_out_d[512:1024, :].rearrange("(k p) c -> p k c", p=128), (), ["WOc"], eng="pool")
    dma(CB[:, :], capb_d[:, 0:32], (), ["CB"])
    dma(LIM[:, :], capb_d[:, 32:64], (), ["LIM"])


    Lb = [P.sb([128, 36], F32) for _ in range(2)]
    junkD = [junk, P.sb([128, D], BF16)]

    def d_front(blk):
        j = blk // 4
        bi = blk % 4
        oms = OMs[j % 2]
        ocs = OCs[j % 2]
        if bi == 0:
            dma(oms[:, :, :], om_s.rearrange("(c two) p t -> (two p) c t", two=2)[:, :, j * 512:(j + 1) * 512], ["om_s"], ["OMs%d" % (j % 2)])
            dma(ocs[:, :, :], ocv_s[:, :, j * 512:(j + 1) * 512].rearrange("c p t -> p c t"), ["ocv_s"], ["OCs%d" % (j % 2)])
        s = blk % 2
        XNF = XNFb[s]
        XNFT = XNFTb[s]
        junk = junkD[s]
        tsl = slice(bi * 128, (bi + 1) * 128)
        dma(XB[s][:, :], x_own[blk * 128:(blk + 1) * 128, :], (), ["Dxb%d" % s])
        for half in range(2):
            pbi = half if blk % 2 == 0 else 6 + half
            pb_ = PS[pbi]
            cs = slice(half * 512, (half + 1) * 512)
            for hh in range(4):
                mm(pb_[:, :], oms[:, hh, tsl], WOm[:, hh, cs], hh == 0, False, ["OMs%d" % (j % 2), "WOm"], ["ps%d" % pbi])
            for cc in range(4):
                mm(pb_[:, :], ocs[:, cc, tsl], WOc[:, cc, cs], False, cc == 3, ["OCs%d" % (j % 2), "WOc"], ["ps%d" % pbi])
            tt("dve", H1b[s][:, cs], XB[s][:, cs], pb_[:, :], ALU.add, ["Dxb%d" % s, "ps%d" % pbi], ["H1b%d" % s])
        dma(h1_s[blk * 128:(blk + 1) * 128, :], H1b[s][:, :], ["H1b%d" % s], ["h1_s"], eng="pool")
        yield
        ss = stats[:, 8 * s + 0:8 * s + 1]
        sd = stats[:, 8 * s + 1:8 * s + 2]
        rs_ = stats[:, 8 * s + 2:8 * s + 3]
        act(junk[:, :], H1b[s][:, :], AF.Square, ["H1b%d" % s], ["junk" + str(s), "st_ss" + str(s)], accum=ss)
        act(sd, ss, AF.Ln, ["st_ss" + str(s), "epsc"], ["st_sd" + str(s)], bias=epsc[:, 0:1], scale=1.0 / D)
        act(rs_, sd, AF.Exp, ["st_sd" + str(s)], ["st_rs" + str(s)], scale=-0.5)
        stt("dve", XNF[:, :], H1b[s][:, :], rs_, gbc[:, :], ALU.mult, ALU.mult, ["H1b%d" % s, "st_rs" + str(s), "gbc"], ["XNF" + str(s)])
        cp("act", XN2[blk % 4][:, :], XNF[:, :], ["XNF" + str(s)], ["XN2%d" % (blk % 4)])
        yield
        for k in range(8):
            pst = PS[2 + k // 4]
            tr(pst[:, (k % 4) * 128:(k % 4 + 1) * 128], XNF[:, k * 128:(k + 1) * 128], ident_f[:, :],
               ["XNF" + str(s), "ident_f"], ["ps%d" % (2 + k // 4)])
        cp("act", XNFT[:, 0:4, :], PS[2][:, :].rearrange("p (k t) -> p k t", k=4), ["ps2"], ["XNFTa" + str(s)])
        cp("dve", XNFT[:, 4:8, :], PS[3][:, :].rearrange("p (k t) -> p k t", k=4), ["ps3"], ["XNFTb" + str(s)])
        yield
        for k in range(8):
            mm(PS[4][:, 0:36], XNFT[:, k, :], WR[:, k, :], k == 0, k == 7, ["XNFTa" + str(s), "XNFTb" + str(s), "WR"], ["ps4"])
        tt("dve", Lb[s][:, :], PS[4][:, 0:36], brt[:, :], ALU.add, ["ps4", "brt"], ["rL%d" % s])
        yield

    def d_tail(blk):
        s = blk % 2
        R = Rb[s]
        A32 = A32b[s]

        def rr(a, b):
            return R[:, a:b]
        pc = 64 * s
        L = Lb[s][:, :]
        gmax = rr(36, 37)
        redmax(gmax, L[:, 0:4], ["rL%d" % s], ["rgmax" + str(s)])
        yield
        G = rr(40, 44)
        ts("dve", G, L[:, 0:4], gmax, None, ALU.is_equal, None, ["rL%d" % s, "rgmax" + str(s)], ["rG" + str(s)])
        yield
        ngmax = rr(37, 38)
        ts("dve", ngmax, gmax, -1.0, None, ALU.mult, None, ["rgmax" + str(s)], ["rngmax" + str(s)])
        yield
        gsum = rr(38, 39)
        act(rr(44, 48), L[:, 0:4], AF.Exp, ["rL%d" % s, "rngmax" + str(s)], ["rgexp" + str(s), "rgsum" + str(s)], bias=ngmax, accum=gsum)
        yield
        gp = rr(39, 40)
        recip(gp, gsum, ["rgsum" + str(s)], ["rgp" + str(s)])
        yield
        el = rr(48, 56)
        ts("dve", el, L[:, 4:12], G[:, 0:1], None, ALU.mult, None, ["rL%d" % s, "rG" + str(s)], ["rel" + str(s)])
        yield
        for g_ in range(1, 4):
            stt("dve", el, L[:, 4 + 8 * g_:12 + 8 * g_], G[:, g_:g_ + 1], el, ALU.mult, ALU.add, ["rL%d" % s, "rG" + str(s), "rel" + str(s)], ["rel" + str(s)])
        m1 = rr(56, 57)
        redmax(m1, el, ["rel" + str(s)], ["rm1" + str(s)])
        yield
        E1 = rr(64, 72)
        ts("dve", E1, el, m1, None, ALU.is_equal, None, ["rel" + str(s), "rm1" + str(s)], ["rE1" + str(s)])
        yield
        el2 = rr(72, 80)
        stt("dve", el2, E1, -1.0e30, el, ALU.mult, ALU.add, ["rE1" + str(s), "rel" + str(s)], ["rel2" + str(s)])
        yield
        m2 = rr(57, 58)
        redmax(m2, el2, ["rel2" + str(s)], ["rm2" + str(s)])
        yield
        E2 = rr(80, 88)
        ts("dve", E2, el2, m2, None, ALU.is_equal, None, ["rel2" + str(s), "rm2" + str(s)], ["rE2" + str(s)])
        yield
        dd = rr(58, 59)
        tt("dve", dd, m2, m1, ALU.subtract, ["rm2" + str(s), "rm1" + str(s)], ["rdd" + str(s)])
        yield
        ed = rr(59, 60)
        act(ed, dd, AF.Exp, ["rdd" + str(s)], ["red" + str(s)])
        yield
        den = rr(60, 61)
        ts("dve", den, ed, 1.0, None, ALU.add, None, ["red" + str(s)], ["rden_" + str(s)])
        yield
        rdn = rr(61, 62)
        recip(rdn, den, ["rden_" + str(s)], ["rrdn" + str(s)])
        yield
        w1 = rr(62, 63)
        tt("dve", w1, gp, rdn, ALU.mult, ["rgp" + str(s), "rrdn" + str(s)], ["rw1" + str(s)])
        yield
        w2 = rr(63, 64)
        tt("dve", w2, w1, ed, ALU.mult, ["rw1" + str(s), "red" + str(s)], ["rw2" + str(s)])
        yield
        A8 = rr(88, 96)
        tt("dve", A8, E1, E2, ALU.add, ["rE1" + str(s), "rE2" + str(s)], ["rA8" + str(s)])
        yield
        for g_ in range(4):
            ts("dve", A32[:, g_ * 8:(g_ + 1) * 8], A8, G[:, g_:g_ + 1], None, ALU.mult, None, ["rA8" + str(s), "rG" + str(s)], ["A32" + str(s)])
        mm(PS[5][:, pc:pc + 32], utri_b[:, :], A32[:, :], True, True, ["utri_b", "A32" + str(s)], ["ps5" + str(s)])
        yield
        mm(PS[5][:, pc + 32:pc + 64], ones_b[:, :], A32[:, :], True, True, ["ones_b", "A32" + str(s)], ["ps5" + str(s)])
        yield
        POSB = rr(96, 128)
        tt("dve", POSB, PS[5][:, pc:pc + 32], CB[:, :], ALU.add, ["ps5" + str(s), "CB"], ["rPOSB" + str(s)])
        yield
        tt("dve", CB[:, :], CB[:, :], PS[5][:, pc + 32:pc + 64], ALU.add, ["ps5" + str(s), "CB", "rPOSB" + str(s)], ["CB"])
        yield
        ovf = rr(128, 160)
        tt("dve", ovf, POSB, LIM[:, :], ALU.is_ge, ["rPOSB" + str(s), "LIM"], ["rovf" + str(s)])
        yield
        stt("dve", POSB, ovf, BIG, POSB, ALU.mult, ALU.add, ["rovf" + str(s), "rPOSB" + str(s)], ["rPOSB" + str(s)])
        yield
        PG = rr(160, 168)
        ts("dve", PG, POSB[:, 0:8], G[:, 0:1], None, ALU.mult, None, ["rPOSB" + str(s), "rG" + str(s)], ["rPG" + str(s)])
        yield
        for g_ in range(1, 4):
            stt("dve", PG, POSB[:, 8 * g_:8 * g_ + 8], G[:, g_:g_ + 1], PG, ALU.mult, ALU.add, ["rPOSB" + str(s), "rG" + str(s), "rPG" + str(s)], ["rPG" + str(s)])
        slf = rr(176, 178)
        tmp8 = rr(168, 176)
        tt("dve", tmp8, E1, PG, ALU.mult, ["rE1" + str(s), "rPG" + str(s)], ["rtmp8" + str(s)])
        yield
        redsum(slf[:, 0:1], tmp8, ["rtmp8" + str(s)], ["rslf0" + str(s)])
        yield
        tt("dve", tmp8, E2, PG, ALU.mult, ["rE2" + str(s), "rPG" + str(s), "rslf0" + str(s)], ["rtmp8" + str(s)])
        yield
        redsum(slf[:, 1:2], tmp8, ["rtmp8" + str(s)], ["rslf1" + str(s)])
        yield
        s4 = blk % 4
        cp("dve", sli[s4][:, :], slf, ["rslf0" + str(s), "rslf1" + str(s)], ["sli%d" % s4])
        yield
        cp("dve", SL[:, blk, :], sli[s4][:, :], ["sli%d" % s4], ["SL"])
        yield
        okm = rr(178, 180)
        ts("dve", okm, slf, float(NSLOT) - 0.5, None, ALU.is_lt, None, ["rslf0" + str(s), "rslf1" + str(s)], ["rokm" + str(s)])
        yield
        tt("dve", WTS[:, blk, 0:1], w1, okm[:, 0:1], ALU.mult, ["rw1" + str(s), "rokm" + str(s)], ["WTS"])
        yield
        tt("dve", WTS[:, blk, 1:2], w2, okm[:, 1:2], ALU.mult, ["rw2" + str(s), "rokm" + str(s)], ["WTS"])
        yield
        for k_ in range(2):
            P.op("pool", (lambda e, s4=s4, k_=k_: e.indirect_dma_start(
                out=xe_s, out_offset=bass.IndirectOffsetOnAxis(ap=sli[s4][:, k_:k_ + 1], axis=0),
                in_=XN2[s4][:, :], in_offset=None, bounds_check=P.breg, oob_is_err=False)),
                ["sli%d" % s4, "XN2%d" % s4], ["xe_s"], dma=True, cost=1.2, lat=6.0)


    exhaust(d_front(0))
    for blk in range(32):
        gens = []
        if blk + 1 < 32:
            gens.append((d_front(blk + 1), 1))
        gens.append((d_tail(blk), 14))
        drive(*gens)

    P.persist_top = persist2
    P.phase_reset()
    WG = [P.sb([128, 8, 512], BF16) for _ in range(2)]
    WU = [P.sb([128, 8, 512], BF16) for _ in range(2)]
    WD = [P.sb([128, 4, D], BF16) for _ in range(2)]
    XEb = [P.sb([128, D], BF16) for _ in range(3)]
    XTe = [P.sb([128, 8, CAP], BF16) for _ in range(2)]
    hT = [P.sb([128, 4, CAP], BF16) for _ in range(2)]
    sg = [P.sb([128, CAP], F32) for _ in range(2)]
    Yb = [P.sb([128, D], F32) for _ in range(2)]
    ycnt = 0
    wload_late = []
    for e_ in range(NE):
        eb = e_ % 2
        if e_ + 2 < NE:
            wload_late.append(e_ + 2)
        for q4 in range(4):
            cp("dve", WG[eb][:, 2 * q4:2 * q4 + 2, :], SG[eb][:, 2 * q4:2 * q4 + 2, :], ["SG%d" % eb], ["WG%d" % eb])
            cp("act", WU[eb][:, 2 * q4:2 * q4 + 2, :], SU[eb][:, 2 * q4:2 * q4 + 2, :], ["SU%d" % eb], ["WU%d" % eb])
            cp("act" if q4 % 2 else "dve", WD[eb][:, q4, :], SD[eb][:, q4, :], ["SD%d" % eb], ["WD%d" % eb])
        while wload_late:
            wload(wload_late.pop(0))
        xte = XTe[eb]
        for b3 in range(3):
            xb_ = XEb[b3]
            r0 = e_ * CAP + b3 * 128
            dma(xb_[:, :], xe_s[r0:r0 + 128, :], ["xe_s"], ["XEb%d" % b3])
            pv = psb(b3 % 2)
            for k in range(8):
                tr(pv[:, k * 128:(k + 1) * 128], xb_[:, k * 128:(k + 1) * 128], ident_b[:, :], ["XEb%d" % b3, "ident_b"],
                   ["ps%d" % (b3 % 2)])
            cp("act" if b3 % 2 else "dve", xte[:, :, b3 * 128:(b3 + 1) * 128],
               pv[:, 0:1024].rearrange("p (k t) -> p k t", k=8), ["ps%d" % (b3 % 2)], ["XTe%d" % eb])
        for fc in range(4):
            pg = 2 + (fc % 2) * 2
            for k in range(8):
                mm(PS[pg][:, 0:CAP], WG[eb][:, k, fc * 128:(fc + 1) * 128], xte[:, k, :], k == 0, k == 7,
                   ["WG%d" % eb, "XTe%d" % eb], ["ps%d" % pg])
            for k in range(8):
                mm(PS[pg + 1][:, 0:CAP], WU[eb][:, k, fc * 128:(fc + 1) * 128], xte[:, k, :], k == 0, k == 7,
                   ["WU%d" % eb, "XTe%d" % eb], ["ps%d" % (pg + 1)])
            act(sg[fc % 2][:, :], PS[pg][:, 0:CAP], AF.Silu, ["ps%d" % pg], ["sg%d" % (fc % 2)])
            tt("dve", hT[eb][:, fc, :], sg[fc % 2][:, :], PS[pg + 1][:, 0:CAP], ALU.mult, ["sg%d" % (fc % 2), "ps%d" % (pg + 1)],
               ["hT%d" % eb])
        for b3 in range(3):
            yb = Yb[ycnt % 2]
            ybn = "Yb%d" % (ycnt % 2)
            for half in range(2):
                pb_ = 6 + half
                for fc in range(4):
                    mm(PS[pb_][:, :], hT[eb][:, fc, b3 * 128:(b3 + 1) * 128], WD[eb][:, fc, half * 512:(half + 1) * 512],
                       fc == 0, fc == 3, ["hT%d" % eb, "WD%d" % eb], ["ps%d" % pb_])
                cp("act" if half else "dve", yb[:, half * 512:(half + 1) * 512], PS[pb_][:, :], ["ps%d" % pb_], [ybn])
            r0 = e_ * CAP + b3 * 128
            dma(ye_s[r0:r0 + 128, :], yb[:, :], [ybn], ["ye_s"], eng="pool")
            ycnt += 1

    P.persist_top = persist_rt
    P.phase_reset()
    junk = P.sb([128, D], BF16)
    gple = P.sb([128, D], F32)
    gfin = P.sb([128, D], F32)
    bple = P.sb([128, D], F32)
    WPG = P.sb([128, 8, D], BF16)
    WPP = P.sb([128, 2, D], BF16)
    H1c = [P.sb([128, D], F32) for _ in range(4)]
    Y1 = [P.sb([128, D], F32) for _ in range(4)]
    Y2 = [P.sb([128, D], F32) for _ in range(4)]
    Pb = [P.sb([128, 256], F32) for _ in range(4)]
    Pbb = P.sb([128, 256], BF16)
    XN3 = P.sb([128, D], BF16)
    XT3 = P.sb([128, 8, 128], BF16)
    PT3 = P.sb([128, 2, 128], BF16)
    tg_ = P.sb([128, D], F32)
    OUTb = [P.sb([128, D], F32) for _ in range(2)]
    negh = P.sb([128, 1], F32)
    memset("pool", negh[:, :], -0.5, ["negh"])
    dma(gple[:, :], g_ple_d, (), ["gple"])
    dma(gfin[:, :], g_fin_d, (), ["gfin"])
    bplb = P.sb([1, D], BF16)
    dma(bplb[:, :], b_ple_d[0:1, :], (), ["bplb"], eng="pool")
    dma(WPG[:, :, :], w_pg_d.rearrange("(k p) c -> p k c", p=128), (), ["WPG"], eng="pool")
    dma(WPP[:, :, :], w_pp_d.rearrange("(k p) c -> p k c", p=128), (), ["WPP"], eng="pool")
    for s in range(4):
        memset("pool", Y1[s][:, :], 0.0, ["Y1%d" % s])
        memset("pool", Y2[s][:, :], 0.0, ["Y2%d" % s])
    XT3b = [XT3] + [P.sb([128, 8, 128], BF16) for _ in range(3)]
    PT3b = [PT3] + [P.sb([128, 2, 128], BF16) for _ in range(3)]
    XN3b = [XN3, P.sb([128, D], BF16)]
    Pbb2 = [Pbb, P.sb([128, 256], BF16)]
    tg2 = [tg_, P.sb([128, D], F32)]
    junk2 = [junk, P.sb([128, D], BF16)]

    def f_a(blk):
        s = blk % 4
        s2 = blk % 4
        pz = blk % 2
        XN3 = XN3b[pz]
        Pbb = Pbb2[pz]
        junk = junk2[pz]
        h = H1c[s]
        hn = "H1c%d" % s
        dma(h[:, :], h1_s[blk * 128:(blk + 1) * 128, :], ["h1_s"], [hn])
        dma(Pb[s][:, :], p_own[blk * 128:(blk + 1) * 128, :], (), ["Pb%d" % s])
        for k_, Yk in enumerate((Y1, Y2)):
            yn = "Y%d%d" % (k_ + 1, s)
            P.op("pool", (lambda e, s=s, k_=k_, Yk=Yk, blk=blk: e.indirect_dma_start(
                out=Yk[s][:, :], out_offset=None, in_=ye_s,
                in_offset=bass.IndirectOffsetOnAxis(ap=SL[:, blk, k_:k_ + 1], axis=0),
                bounds_check=P.breg, oob_is_err=False)), ["SL", "ye_s"], [yn], dma=True, cost=1.2, lat=8.0)
        yield
        stt("dve", h[:, :], Y1[s][:, :], WTS[:, blk, 0:1], h[:, :], ALU.mult, ALU.add, ["Y1%d" % s, "WTS", hn], [hn])
        stt("dve", h[:, :], Y2[s][:, :], WTS[:, blk, 1:2], h[:, :], ALU.mult, ALU.add, ["Y2%d" % s, "WTS", hn], [hn])
        yield
        ss = stats[:, 8 * pz + 0:8 * pz + 1]
        sd = stats[:, 8 * pz + 1:8 * pz + 2]
        rs_ = stats[:, 8 * pz + 2:8 * pz + 3]
        act(junk[:, :], h[:, :], AF.Square, [hn], ["junk" + str(pz), "st_ss" + str(pz)], accum=ss)
        ts("pool", sd, ss, 1.0 / D, EPS, ALU.mult, ALU.add, ["st_ss" + str(pz)], ["st_sd" + str(pz)])
        tt("pool", rs_, sd, negh[:, 0:1], ALU.pow, ["st_sd" + str(pz), "negh"], ["st_rs" + str(pz)])
        stt("dve", XN3[:, :], h[:, :], rs_, gple[:, :], ALU.mult, ALU.mult, [hn, "st_rs" + str(pz), "gple"], ["XN3" + str(pz)])
        yield
        pv = psb(0)
        for k in range(8):
            tr(pv[:, k * 128:(k + 1) * 128], XN3[:, k * 128:(k + 1) * 128], ident_b[:, :], ["XN3" + str(pz), "ident_b"], ["ps0"])
        cp("act", XT3b[s2][:, :, :], pv[:, 0:1024].rearrange("p (k t) -> p k t", k=8), ["ps0"], ["XT3%d" % s2])
        yield
        cp("act", Pbb[:, :], Pb[s][:, :], ["Pb%d" % s], ["Pbb" + str(pz)])
        pv1 = psb(1)
        for k in range(2):
            tr(pv1[:, k * 128:(k + 1) * 128], Pbb[:, k * 128:(k + 1) * 128], ident_b[:, :], ["Pbb" + str(pz), "ident_b"], ["ps1"])
        cp("dve", PT3b[s2][:, :, :], pv1[:, 0:256].rearrange("p (k t) -> p k t", k=2), ["ps1"], ["PT3%d" % s2])
        yield

    def f_b(blk):
        s = blk % 2
        s3 = blk % 4
        pz = blk % 2
        tg_ = tg2[pz]
        junk = junk2[pz]
        h = H1c[s3]
        hn = "H1c%d" % s3
        for half in range(2):
            cs = slice(half * 512, (half + 1) * 512)
            pg_ = (2 + half) if blk % 2 == 0 else (6 + half)
            pp_ = 4 + half
            for k in range(8):
                mm(PS[pg_][:, :], XT3b[s3][:, k, :], WPG[:, k, cs], k == 0, False, ["XT3%d" % s3, "WPG"], ["ps%d" % pg_])
            mm(PS[pg_][:, :], ones_b[0:1, :], bplb[0:1, cs], False, True, ["ones_b", "bplb"], ["ps%d" % pg_])
            for k in range(2):
                mm(PS[pp_][:, :], PT3b[s3][:, k, :], WPP[:, k, cs], k == 0, k == 1, ["PT3%d" % s3, "WPP"], ["ps%d" % pp_])
            yield
            act(tg_[:, cs], PS[pg_][:, :], AF.Tanh, ["ps%d" % pg_], ["tg%d_%d" % (half, pz)], scale=0.5)
            stt("dve", tg_[:, cs], tg_[:, cs], 1.0, PS[pp_][:, :], ALU.add, ALU.mult, ["tg%d_%d" % (half, pz), "ps%d" % pp_], ["tg%d_%d" % (half, pz)])
            stt("dve", h[:, cs], tg_[:, cs], 0.5, h[:, cs], ALU.mult, ALU.add, [hn, "tg%d_%d" % (half, pz)], [hn])
            yield
        ss2 = stats[:, 8 * pz + 4:8 * pz + 5]
        sd2 = stats[:, 8 * pz + 5:8 * pz + 6]
        rs2 = stats[:, 8 * pz + 6:8 * pz + 7]
        act(junk[:, :], h[:, :], AF.Square, [hn], ["junk" + str(pz), "st_ss2" + str(pz)], accum=ss2)
        ts("pool", sd2, ss2, 1.0 / D, EPS, ALU.mult, ALU.add, ["st_ss2" + str(pz)], ["st_sd2" + str(pz)])
        tt("pool", rs2, sd2, negh[:, 0:1], ALU.pow, ["st_sd2" + str(pz), "negh"], ["st_rs2" + str(pz)])
        yield
        ob = OUTb[s]
        stt("dve", ob[:, :], h[:, :], rs2, gfin[:, :], ALU.mult, ALU.mult, [hn, "st_rs2" + str(pz), "gfin"], ["OUTb%d" % s])
        dma(out_d[blk * 128:(blk + 1) * 128, :], ob[:, :], ["OUTb%d" % s], ["out_d"], eng="pool")

    exhaust(f_a(0))
    exhaust(f_a(1))
    exhaust(f_a(2))
    for blk in range(32):
        gens = []
        if blk + 3 < 32:
            gens.append((f_a(blk + 3), 1))
        gens.append((f_b(blk), 1))
        drive(*gens)
    P.barrier()

    P.emit(stack)
    stack.close()
    return nc


_NC = None


def _perm32():
    return np.concatenate([np.arange(16, 32), np.arange(0, 16)])


def kernel(x, p, positions, attn_norm_g, w_in, q_norm_g, w_uq, kv_norm_g, w_ukv, conv_w, w_out, moe_norm_g,
           w_group_router, b_group_router, w_expert_router, b_expert_router, w_gate, w_up, w_down, ple_norm_g,
           w_ple_gate, b_ple_gate, w_ple_proj, final_norm_g):
    global _NC
    f = np.float32
    x = np.asarray(x, f)
    p = np.asarray(p, f)
    positions = np.asarray(positions, np.int32)
    w_in0 = np.ascontiguousarray(np.asarray(w_in, f)[0])
    perm = _perm32()
    w_uq0 = np.asarray(w_uq, f)[0]
    w_ukv0 = np.asarray(w_ukv, f)[0]

    def bc(v, n=128):
        return np.ascontiguousarray(np.broadcast_to(np.asarray(v, f).reshape(1, -1), (n, np.asarray(v).size)))

    inv_freq = (10000.0 ** (-np.arange(0, 32, 2, dtype=np.float32) / 32.0)).astype(f)
    rconst = np.zeros((32, 4), f)
    rconst[:, 2] = np.pi / 2
    rconst[:, 0] = np.concatenate([inv_freq, inv_freq])
    rconst[:16, 1] = -1.0
    rconst[16:, 1] = 1.0
    esel = np.zeros((65, 64), f)
    esel[64, :] = 1.0
    capb = np.zeros((128, 64), f)
    capb[:, 0:32] = (np.arange(32) * CAP)[None, :]
    capb[:, 32:64] = ((np.arange(32) + 1) * CAP)[None, :]
    shared = {
        "iota": bc(np.arange(512, dtype=f)),
        "ident": np.eye(128, dtype=f),
        "utri": np.triu(np.ones((128, 128), f), 1),
        "rconst": rconst,
        "esel": esel,
        "capb": capb,
        "g_attn": bc(attn_norm_g[0]),
        "g_moe": bc(moe_norm_g[0]),
        "g_ple": bc(ple_norm_g[0]),
        "g_fin": bc(final_norm_g),
        "b_ple": bc(b_ple_gate[0]),
        "b_rt": bc(np.concatenate([np.asarray(b_group_router, f)[0], np.asarray(b_expert_router, f)[0]])),
        "gq_t": np.ascontiguousarray(np.asarray(q_norm_g, f)[0].reshape(3, 128).T),
        "gkv_t": np.ascontiguousarray(np.asarray(kv_norm_g, f)[0].reshape(2, 128).T),
        "cw_t": np.ascontiguousarray(np.asarray(conv_w, f)[0][:, 0, :].reshape(3, 4, 128).transpose(2, 1, 0).reshape(128, 12)),
        "w_in": w_in0,
        "w_krp": np.ascontiguousarray(w_in0[:, 640 + perm]),
        "w_uqn": np.ascontiguousarray(w_uq0[:, :, 0:64].reshape(QL, NH * 64)),
        "w_uqr": np.ascontiguousarray(w_uq0[:, :, 64:96].reshape(QL, NH * 32)),
        "w_uqp": np.ascontiguousarray(w_uq0[:, :, 64 + perm].reshape(QL, NH * 32)),
        "w_ukn": np.ascontiguousarray(w_ukv0[:, :, 0:64].reshape(KVL, NH * 64)),
        "w_uv": np.ascontiguousarray(w_ukv0[:, :, 64:128].reshape(KVL, NH * 64)),
        "w_out": np.ascontiguousarray(np.asarray(w_out, f)[0]),
        "w_rt": np.ascontiguousarray(np.concatenate([np.asarray(w_group_router, f)[0], np.asarray(w_expert_router, f)[0]], axis=1)),
        "w_gate": np.ascontiguousarray(np.asarray(w_gate, f)[0]),
        "w_up": np.ascontiguousarray(np.asarray(w_up, f)[0]),
        "w_down": np.ascontiguousarray(np.asarray(w_down, f)[0]),
        "w_pg": np.ascontiguousarray(np.asarray(w_ple_gate, f)[0]),
        "w_pp": np.ascontiguousarray(np.asarray(w_ple_proj, f)[0]),
    }
    in_maps = []
    owns = []
    for c in range(8):
        b = c // 2
        hf = c % 2
        T = TILES[hf]
        own = np.concatenate([np.arange(t * 512, (t + 1) * 512) for t in T])
        owns.append(own)
        halo = np.zeros((16, D), f)
        for j, t in enumerate(T):
            if t > 0:
                halo[2 * j:2 * j + 2] = x[b, t * 512 - 2:t * 512]
        kthr = np.zeros((128, 64), f)
        for j, t in enumerate(T):
            delta = t - 2 * j
            for r_ in range(8):
                kthr[:, j * 8 + r_] = r_ * 128 + np.arange(128) - 512 * delta
        m = dict(shared)
        m["x_all"] = np.ascontiguousarray(x[b])
        m["x_own"] = np.ascontiguousarray(x[b][own])
        m["x_halo"] = halo
        m["p_own"] = np.ascontiguousarray(p[0, b][own])
        m["pos_all"] = np.ascontiguousarray(np.broadcast_to(positions[b][None, :], (32, S)))
        m["pos_own"] = np.ascontiguousarray(np.broadcast_to(positions[b][own][None, :], (32, NT)))
        m["kthr"] = kthr
        in_maps.append(m)
    if _NC is None:
        _NC = build_program()
    res = run_bass_kernel_spmd(_NC, in_maps, core_ids=list(range(8)))
    out = np.zeros((4, S, D), f)
    for c in range(8):
        out[c // 2, owns[c]] = np.asarray(res.results[c]["out"], f)
    return out
```
